# Optimizing a Trainium2 kernel written in Bass

```python
import functools
import jax, jax.numpy as jnp
from jax import lax
import numpy as np

D_MODEL = 1024
BATCH = 8
SEQ = 2048
DEPTH = 2
DEC_BATCH = 32
DEC_SEQ = 4
PAST_LEN = 8192
PAGE_SIZE = 128

HEAD_DIM = 64
D_MIX = D_MODEL
RET_HEADS = 6
MOBA_HEADS = 6
POOL_GROUPS = 4
RET_W = RET_HEADS * HEAD_DIM
MOBA_W = MOBA_HEADS * HEAD_DIM
POOL_W = D_MIX - RET_W - MOBA_W
POOL_GC = POOL_W // POOL_GROUPS
POOL_WINDOWS = (2, 4, 8, 16)
POOL_BUF = 15
RET_CHUNK = 128
MOBA_BLOCK = 256
MOBA_TOPK = 3
MOBA_QBLK = 32
ROPE_THETA = 10000.0
EPS = 1e-6
D_IN = 4 * RET_W + 4 * MOBA_W + 2 * POOL_W

kernel_name = "hybrid_retention_moba_pool_decode_step"

F32 = jnp.float32


def _split_points():
    sizes = [RET_W] * 4 + [MOBA_W] * 4 + [POOL_W] * 2
    return [int(s) for s in np.cumsum(sizes)[:-1]]


def _rmsnorm(x, w):
    xf = x.astype(F32)
    return xf * lax.rsqrt(jnp.mean(xf * xf, axis=-1, keepdims=True) + EPS) * w.astype(F32)


def _heads(t):
    return t.reshape(t.shape[:-1] + (-1, HEAD_DIM))


def _rope(x, pos):
    half = HEAD_DIM // 2
    inv = 1.0 / (ROPE_THETA ** (jnp.arange(half, dtype=F32) / half))
    ang = pos.astype(F32)[:, None] * inv[None, :]
    cos = jnp.cos(ang)[:, None, :]
    sin = jnp.sin(ang)[:, None, :]
    x1, x2 = x[..., :half], x[..., half:]
    return jnp.concatenate([x1 * cos - x2 * sin, x2 * cos + x1 * sin], axis=-1)


def _ret_chunk(S, q, k, v, log_g):
    L = q.shape[2]
    i = jnp.arange(L, dtype=F32)
    rel = i[:, None] - i[None, :]
    lg = log_g[:, None, None]
    decay = jnp.where(rel[None] >= 0, jnp.exp(rel[None] * lg), 0.0)
    inner = jnp.einsum('bhid,bhjd->bhij', q, k) * decay
    o = jnp.einsum('bhij,bhjd->bhid', inner, v)
    o = o + jnp.einsum('bhid,bhde->bhie', q, S) * jnp.exp((i + 1.0)[None, :, None] * lg)
    k_dec = k * jnp.exp((L - 1.0 - i)[None, :, None] * lg)
    S_new = S * jnp.exp(L * log_g)[:, None, None] + jnp.einsum('bhjd,bhje->bhde', k_dec, v)
    return S_new, o


def _retention(q, k, v, S0, gn_w):
    B, L, H, d = q.shape
    log_g = jnp.log(1.0 - 2.0 ** (-5.0 - jnp.arange(H, dtype=F32)))
    c = RET_CHUNK if L % RET_CHUNK == 0 else L
    n = L // c

    def chunks(t):
        return t.astype(F32).reshape(B, n, c, H, d).transpose(1, 0, 3, 2, 4)

    S, o = lax.scan(lambda s, xs: _ret_chunk(s, xs[0], xs[1], xs[2], log_g),
                    S0.astype(F32), (chunks(q), chunks(k), chunks(v)))
    o = o.transpose(1, 0, 3, 2, 4).reshape(B, L, H, d)
    mu = jnp.mean(o, axis=-1, keepdims=True)
    var = jnp.mean(jnp.square(o - mu), axis=-1, keepdims=True)
    o = (o - mu) * lax.rsqrt(var + EPS) * gn_w.astype(F32).reshape(H, d)
    return o.reshape(B, L, H * d), S


def _moba_core(q, q_pos, k_blocks, v_blocks, k_mean, n_past, k_own, v_own, own_pos):
    B, H, Q, _ = q.shape
    scale = HEAD_DIM ** -0.5
    s_own = jnp.einsum('bhqd,bhkd->bhqk', q, k_own).astype(F32) * scale
    s_own = jnp.where(own_pos[None, :] <= q_pos[:, None], s_own, -jnp.inf)
    n_c = k_blocks.shape[2]
    k_sel = min(MOBA_TOPK, n_c)
    if k_sel == 0:
        p = jax.nn.softmax(s_own, axis=-1)
        return jnp.einsum('bhqk,bhkd->bhqd', p, v_own.astype(F32))
    gate = jnp.einsum('bhqd,bhnd->bhqn', q.astype(F32), k_mean)
    gate = jnp.where(jnp.arange(n_c) < n_past, gate, -jnp.inf)
    _, idx = lax.top_k(gate, k_sel)
    gather = jax.vmap(jax.vmap(lambda blk, ix: blk[ix]))
    k_g = gather(k_blocks, idx)
    v_g = gather(v_blocks, idx)
    s_sel = jnp.einsum('bhqd,bhqjkd->bhqjk', q, k_g).astype(F32) * scale
    s_sel = jnp.where((jnp.arange(k_sel) < n_past)[:, None], s_sel, -jnp.inf)
    n_sel = k_sel * MOBA_BLOCK
    p = jax.nn.softmax(jnp.concatenate([s_sel.reshape(B, H, Q, n_sel), s_own], axis=-1), axis=-1)
    o = jnp.einsum('bhqjk,bhqjkd->bhqd', p[..., :n_sel].reshape(B, H, Q, k_sel, MOBA_BLOCK), v_g.astype(F32))
    return o + jnp.einsum('bhqk,bhkd->bhqd', p[..., n_sel:], v_own.astype(F32))


def _moba_prompt(q, k, v):
    B, S, H, d = q.shape
    nb = -(-S // MOBA_BLOCK)
    pad = nb * MOBA_BLOCK - S
    qt = q.transpose(0, 2, 1, 3)
    padw = ((0, 0), (0, 0), (0, pad), (0, 0))
    kb = jnp.pad(k.transpose(0, 2, 1, 3), padw).reshape(B, H, nb, MOBA_BLOCK, d)
    vb = jnp.pad(v.transpose(0, 2, 1, 3), padw).reshape(B, H, nb, MOBA_BLOCK, d)
    k_cand, v_cand = kb[:, :, :nb - 1], vb[:, :, :nb - 1]
    k_mean = jnp.mean(k_cand.astype(F32), axis=3)

    def one_block(i):
        q0 = i * MOBA_QBLK
        qb = lax.dynamic_slice_in_dim(qt, q0, MOBA_QBLK, axis=2)
        q_pos = q0 + jnp.arange(MOBA_QBLK)
        own = q0 // MOBA_BLOCK
        k_own = lax.dynamic_index_in_dim(kb, own, axis=2, keepdims=False)
        v_own = lax.dynamic_index_in_dim(vb, own, axis=2, keepdims=False)
        own_pos = own * MOBA_BLOCK + jnp.arange(MOBA_BLOCK)
        return _moba_core(qb, q_pos, k_cand, v_cand, k_mean, own, k_own, v_own, own_pos)

    o = lax.map(one_block, jnp.arange(S // MOBA_QBLK))
    return o.transpose(1, 0, 3, 2, 4).reshape(B, S, H * d)


def _moba_sample(q, k, v, cache_k_l, cache_v_l, page_table, past_len):
    DB, T, H, d = q.shape
    kp = cache_k_l[page_table].reshape(DB, past_len, H, d).transpose(0, 2, 1, 3)
    vp = cache_v_l[page_table].reshape(DB, past_len, H, d).transpose(0, 2, 1, 3)
    n_full = past_len // MOBA_BLOCK
    own_start = n_full * MOBA_BLOCK
    kb = kp[:, :, :own_start].reshape(DB, H, n_full, MOBA_BLOCK, d)
    vb = vp[:, :, :own_start].reshape(DB, H, n_full, MOBA_BLOCK, d)
    k_mean = jnp.mean(kb.astype(F32), axis=3)
    k_own = jnp.concatenate([kp[:, :, own_start:].astype(F32), k.transpose(0, 2, 1, 3).astype(F32)], axis=2)
    v_own = jnp.concatenate([vp[:, :, own_start:].astype(F32), v.transpose(0, 2, 1, 3).astype(F32)], axis=2)
    own_pos = own_start + jnp.arange(k_own.shape[2])
    q_pos = past_len + jnp.arange(T)
    o = _moba_core(q.transpose(0, 2, 1, 3), q_pos, kb, vb, k_mean, n_full, k_own, v_own, own_pos)
    return o.transpose(0, 2, 1, 3).reshape(DB, T, H * d)


def _pool_mixer(u, buf, pos0, w_pool, b_pool, scale):
    L = u.shape[1]
    u_full = jnp.concatenate([buf.astype(F32), u.astype(F32)], axis=1)
    cs = jnp.cumsum(jnp.pad(u_full, ((0, 0), (1, 0), (0, 0))), axis=1)
    pos = pos0 + jnp.arange(L)
    outs = []
    for g, w in enumerate(POOL_WINDOWS):
        sl = slice(g * POOL_GC, (g + 1) * POOL_GC)
        win_sum = cs[:, POOL_BUF + 1:, sl] - cs[:, POOL_BUF + 1 - w:POOL_BUF + 1 - w + L, sl]
        cnt = jnp.minimum(pos + 1, w).astype(F32)[None, :, None]
        pooled = win_sum / cnt - u_full[:, POOL_BUF:, sl]
        outs.append(jnp.einsum('blc,cd->bld', pooled, w_pool[g]) + b_pool[g])
    y = jnp.concatenate(outs, axis=-1) * scale
    return y, u_full[:, -POOL_BUF:]


def _layer(x, pos0, S0, pool_buf, norm_w, w_in, w_out, ret_gn_w, q_norm_w, k_norm_w,
           pool_w, pool_b, pool_scale, moba_attend):
    L = x.shape[1]
    pos = pos0 + jnp.arange(L)
    h = _rmsnorm(x, norm_w)
    z = h @ w_in
    rq, rk, rv, rg, mq, mk, mv, mg, pu, pg = jnp.split(z, _split_points(), axis=-1)
    rq = _rope(_heads(rq), pos)
    rk = _rope(_heads(rk), pos) * (HEAD_DIM ** -0.5)
    r_o, S_new = _retention(rq, rk, _heads(rv), S0, ret_gn_w)
    mq = _rope(_rmsnorm(_heads(mq), q_norm_w), pos)
    mk = _rope(_rmsnorm(_heads(mk), k_norm_w), pos)
    mv = _heads(mv)
    m_o = moba_attend(mq, mk, mv)
    p_o, buf_new = _pool_mixer(pu, pool_buf, pos0, pool_w, pool_b, pool_scale)
    mix = jnp.concatenate([r_o * jax.nn.silu(rg), m_o * jax.nn.silu(mg), p_o * jax.nn.silu(pg)], axis=-1)
    y = x + (mix @ w_out).astype(x.dtype)
    return y, S_new, mk, mv, buf_new


def setup_inputs(seed: int = 0) -> dict:
    key = jax.random.key(seed)
    ks = jax.random.split(key, 16)
    n_pages = PAST_LEN // PAGE_SIZE
    n_pool = (DEC_BATCH * n_pages * 5) // 4
    page_table = jax.random.permutation(ks[0], n_pool)[:DEC_BATCH * n_pages].reshape(DEC_BATCH, n_pages).astype(jnp.int32)
    nrm = jax.random.normal
    return {
        "x_prompt": nrm(ks[1], (BATCH, SEQ, D_MODEL), F32),
        "x_sample": nrm(ks[2], (DEC_BATCH, DEC_SEQ, D_MODEL), F32),
        "cache_k": nrm(ks[3], (DEPTH, n_pool, PAGE_SIZE, MOBA_HEADS, HEAD_DIM), F32),
        "cache_v": nrm(ks[4], (DEPTH, n_pool, PAGE_SIZE, MOBA_HEADS, HEAD_DIM), F32),
        "state_ret": nrm(ks[5], (DEPTH, DEC_BATCH, RET_HEADS, HEAD_DIM, HEAD_DIM), F32),
        "state_pool": nrm(ks[6], (DEPTH, DEC_BATCH, POOL_BUF, POOL_W), F32),
        "page_table": page_table,
        "norm_w": 1.0 + 0.02 * nrm(ks[7], (DEPTH, D_MODEL), F32),
        "w_in": nrm(ks[8], (DEPTH, D_MODEL, D_IN), F32) * D_MODEL ** -0.5,
        "w_out": nrm(ks[9], (DEPTH, D_MIX, D_MODEL), F32) * D_MIX ** -0.5,
        "ret_gn_w": 1.0 + 0.02 * nrm(ks[10], (DEPTH, RET_W), F32),
        "q_norm_w": 1.0 + 0.02 * nrm(ks[11], (DEPTH, HEAD_DIM), F32),
        "k_norm_w": 1.0 + 0.02 * nrm(ks[12], (DEPTH, HEAD_DIM), F32),
        "pool_w": nrm(ks[13], (DEPTH, POOL_GROUPS, POOL_GC, POOL_GC), F32) * POOL_GC ** -0.5,
        "pool_b": 0.02 * nrm(ks[14], (DEPTH, POOL_GROUPS, POOL_GC), F32),
        "pool_scale": 1.0 + 0.1 * nrm(ks[15], (DEPTH, POOL_W), F32),
    }


def reference(x_prompt, x_sample, cache_k, cache_v, state_ret, state_pool, page_table,
              norm_w, w_in, w_out, ret_gn_w, q_norm_w, k_norm_w, pool_w, pool_b, pool_scale):
    past_len = page_table.shape[1] * cache_k.shape[2]
    B = x_prompt.shape[0]
    xp, xs = x_prompt, x_sample
    kps, vps, kss, vss, rps, rss, pps, pss = [], [], [], [], [], [], [], []
    for l in range(DEPTH):
        shared = (norm_w[l], w_in[l], w_out[l], ret_gn_w[l], q_norm_w[l], k_norm_w[l],
                  pool_w[l], pool_b[l], pool_scale[l])
        S0 = jnp.zeros((B, RET_HEADS, HEAD_DIM, HEAD_DIM), F32)
        buf0 = jnp.zeros((B, POOL_BUF, POOL_W), F32)
        xp, S_p, k_p, v_p, b_p = _layer(xp, 0, S0, buf0, *shared, moba_attend=_moba_prompt)
        moba_s = functools.partial(_moba_sample, cache_k_l=cache_k[l], cache_v_l=cache_v[l],
                                   page_table=page_table, past_len=past_len)
        xs, S_s, k_s, v_s, b_s = _layer(xs, past_len, state_ret[l], state_pool[l], *shared, moba_attend=moba_s)
        kps.append(k_p); vps.append(v_p); kss.append(k_s); vss.append(v_s)
        rps.append(S_p); rss.append(S_s); pps.append(b_p); pss.append(b_s)
    return (xp, xs, jnp.stack(kps), jnp.stack(vps), jnp.stack(kss), jnp.stack(vss),
            jnp.stack(rps), jnp.stack(rss), jnp.stack(pps), jnp.stack(pss))
```

```python
import numpy as np
import ml_dtypes
from contextlib import ExitStack
import concourse.bass as bass
import concourse.mybir as mybir
from concourse.bass_utils import run_bass_kernel_spmd

F32 = mybir.dt.float32
BF16 = mybir.dt.bfloat16
I32 = mybir.dt.int32
ALU = mybir.AluOpType
AF = mybir.ActivationFunctionType
AX = mybir.AxisListType

NCORES = 8
D = 1024
SEQ = 2048
NT = SEQ // 128
DEPTH = 2
DIN = 3584
HD = 64
NH = 6
NPOOL = 2560
NPAGES = 64
EPS = 1e-6
NEG = -30000.0
SAME_SYNC = True

O_RQ, O_RK, O_RV, O_MQ, O_MK, O_MV, O_PU, O_RG, O_MG, O_PG = 0, 384, 768, 1152, 1536, 1920, 2304, 2560, 2944, 3328
WIN_SEGS = ((0, 1152, 0), (1152, 384, 2560), (1536, 1152, 1152), (2688, 384, 2944), (3072, 256, 2304), (3328, 256, 3328))


DEF_COST = {"pe": 0.09, "act": 0.55, "dve": 0.6, "pool": 1.0, "sp": 0.1}
SYNC_LAT = 0.15
PRIORITY_MODE = "rank"


class Sched:
    def __init__(self, nc, stack):
        self.nc = nc
        self.stack = stack
        self.names = ["pe", "act", "dve", "pool", "sp"]
        self.sem = {n: stack.enter_context(nc.semaphore("s_" + n)) for n in self.names}
        self.dsem = {}
        self.nodes = []
        self.res_w = {}
        self.res_r = {}
        self.pending = None
        self.epoch = 0
        self.last_dma_of_key = {}
        self.pool_dmas = []

    def _new_node(self, eng, kind):
        nd = dict(id=len(self.nodes), eng=eng, kind=kind, fns=[], cost=0.0, reads=set(), writes=set(),
                  preds=set(), epoch=self.epoch, key=None, val=0, lat=0.0)
        return nd

    def _finalize(self, nd):
        reads = set(nd["reads"])
        writes = set(nd["writes"])
        for r in list(reads):
            if r.startswith("pb"):
                writes.add(r)
        preds = nd["preds"]
        for r in reads:
            w = self.res_w.get(r)
            if w is not None:
                preds.add(w)
        for w_ in writes:
            w = self.res_w.get(w_)
            if w is not None:
                preds.add(w)
            for t in self.res_r.get(w_, ()):
                preds.add(t)
        nd["id"] = len(self.nodes)
        preds.discard(nd["id"])
        self.nodes.append(nd)
        for r in reads:
            self.res_r.setdefault(r, []).append(nd["id"])
        for w_ in writes:
            self.res_w[w_] = nd["id"]
            self.res_r[w_] = []

    def op(self, eng, fn, reads=(), writes=(), mark=True, c=None):
        if self.pending is not None and self.pending["eng"] != eng:
            raise RuntimeError("unmarked group interrupted")
        nd = self.pending if self.pending is not None else self._new_node(eng, "op")
        nd["fns"].append((fn, mark))
        nd["cost"] += (c if c is not None else DEF_COST[eng])
        nd["reads"].update(reads)
        nd["writes"].update(writes)
        if mark:
            self.pending = None
            self._finalize(nd)
        else:
            self.pending = nd

    def dma(self, q, fn, reads=(), writes=(), key=None, c=None, lat=None):
        assert self.pending is None
        k = key if key is not None else (writes[0] if writes else reads[0])
        if k not in self.dsem:
            s = self.stack.enter_context(self.nc.semaphore("d%d" % len(self.dsem)))
            self.dsem[k] = [s, 0]
        ent = self.dsem[k]
        ent[1] += 16
        nd = self._new_node(q, "dma")
        nd["fns"].append((fn, False))
        nd["cost"] = c if c is not None else (1.2 if q == "pool" else 0.1)
        nd["lat"] = lat if lat is not None else 3.0
        nd["reads"].update(reads)
        nd["writes"].update(writes)
        nd["key"] = k
        nd["val"] = ent[1]
        prev = self.last_dma_of_key.get(k)
        if prev is not None:
            nd["preds"].add(("issue", prev))
        if q == "pool":
            if len(self.pool_dmas) >= 10:
                nd["preds"].add(self.pool_dmas[-10])
        self._finalize(nd)
        self.last_dma_of_key[k] = nd["id"]
        if q == "pool":
            self.pool_dmas.append(nd["id"])

    def barrier(self):
        assert self.pending is None
        self.epoch += 1

    def _schedule(self):
        import heapq
        nodes = self.nodes
        n = len(nodes)
        succs = [[] for _ in range(n)]
        npred = [0] * n
        for nd in nodes:
            ps = set()
            for p in nd["preds"]:
                if isinstance(p, tuple):
                    ps.add((p[1], True))
                else:
                    ps.add((p, False))
            norm = {p for p, iss in ps if not iss}
            plist = [(p, False) for p in norm] + [(p, True) for p, iss in ps if iss and p not in norm]
            nd["plist"] = plist
            npred[nd["id"]] = len(plist)
            for p, iss in plist:
                succs[p].append((nd["id"], iss))
        rank = [0.0] * n
        for nd in reversed(nodes):
            i = nd["id"]
            best = 0.0
            for sidx, iss in succs[i]:
                if nodes[sidx]["epoch"] == nd["epoch"] and rank[sidx] > best:
                    best = rank[sidx]
            rank[i] = best + nd["cost"] + (nd["lat"] if nd["kind"] == "dma" else 0.0)
        PRI = PRIORITY_MODE
        order = {e: [] for e in self.names}
        ready_t = [0.0] * n
        eng_free = {e: 0.0 for e in self.names}
        fut = {e: [] for e in self.names}
        rdy = {e: [] for e in self.names}
        epoch_nodes = {}
        for nd in nodes:
            epoch_nodes.setdefault(nd["epoch"], []).append(nd["id"])
        t_base = 0.0
        dma_free = 0.0
        fin = [0.0] * n
        issue_fin = [0.0] * n
        for ep in sorted(epoch_nodes):
            ids = epoch_nodes[ep]
            idset = set(ids)
            left = len(ids)
            for e in self.names:
                eng_free[e] = max(eng_free[e], t_base)
            for i in ids:
                cnt = 0
                for p, iss in nodes[i]["plist"]:
                    if p in idset:
                        cnt += 1
                npred[i] = cnt
                ready_t[i] = t_base
                if cnt == 0:
                    heapq.heappush(fut[nodes[i]["eng"]], (t_base, i))
            while left > 0:
                best = None
                for e in self.names:
                    f = fut[e]
                    r = rdy[e]
                    while f and f[0][0] <= eng_free[e]:
                        j_ = heapq.heappop(f)[1]
                        heapq.heappush(r, ((-rank[j_], j_) if PRI == "rank" else (0.0, j_)))
                    if r:
                        cand = (eng_free[e], r[0][1], e, True)
                    elif f:
                        cand = (f[0][0], f[0][1], e, False)
                    else:
                        continue
                    if best is None or cand[:2] < best[:2]:
                        best = cand
                assert best is not None, "scheduler stuck (dependency cycle?)"
                start, i, e, from_r = best
                if from_r:
                    heapq.heappop(rdy[e])
                else:
                    heapq.heappop(fut[e])
                nd = nodes[i]
                order[e].append(i)
                issue_fin[i] = start + nd["cost"]
                eng_free[e] = issue_fin[i]
                if nd["kind"] == "dma":
                    xfer = max(issue_fin[i], dma_free) + 0.4
                    dma_free = xfer
                    fin[i] = xfer + nd["lat"]
                else:
                    fin[i] = issue_fin[i]
                left -= 1
                for sidx, iss in succs[i]:
                    if sidx not in idset:
                        continue
                    sn = nodes[sidx]
                    t = issue_fin[i] if iss else fin[i]
                    if sn["eng"] != e or nd["kind"] == "dma":
                        t += SYNC_LAT
                    if t > ready_t[sidx]:
                        ready_t[sidx] = t
                    npred[sidx] -= 1
                    if npred[sidx] == 0:
                        heapq.heappush(fut[sn["eng"]], (ready_t[sidx], sidx))
            t_base = max([t_base] + [fin[i] for i in ids])
        self.makespan = t_base
        return order

    def emit(self):
        nc = self.nc
        assert self.pending is None
        nodes = self.nodes
        order = self._schedule()
        cnt = {}
        for e in self.names:
            c = 0
            for i in order[e]:
                if nodes[i]["kind"] == "op":
                    c += 1
                    cnt[i] = c
        ep_eng = {}
        ep_dma = {}
        for nd in nodes:
            ep = nd["epoch"]
            if nd["kind"] == "op":
                d = ep_eng.setdefault(ep, {})
                d[nd["eng"]] = max(d.get(nd["eng"], 0), cnt[nd["id"]])
            else:
                d = ep_dma.setdefault(ep, {})
                d[nd["key"]] = max(d.get(nd["key"], 0), nd["val"])
        prog = {e: [] for e in self.names}
        for e in self.names:
            waited = {}
            cur_ep = 0

            def need(kind, src, val):
                if kind == "eng" and src == e and (e in ("pe", "sp") or not SAME_SYNC):
                    return
                if val <= 0:
                    return
                k = (kind, src)
                if waited.get(k, 0) >= val:
                    return
                waited[k] = val
                prog[e].append(("wait", kind, src, val))

            for i in order[e]:
                nd = nodes[i]
                if nd["epoch"] != cur_ep:
                    for ep in range(cur_ep, nd["epoch"]):
                        for src, v in ep_eng.get(ep, {}).items():
                            if src != e:
                                need("eng", src, v)
                        for k, v in ep_dma.get(ep, {}).items():
                            need("dma", k, v)
                    cur_ep = nd["epoch"]
                for p, iss in nd["plist"]:
                    if iss:
                        continue
                    pn = nodes[p]
                    if pn["kind"] == "op":
                        need("eng", pn["eng"], cnt[p])
                    else:
                        need("dma", pn["key"], pn["val"])
                if nd["kind"] == "op":
                    for fn, mark in nd["fns"]:
                        prog[e].append(("op", fn, mark))
                else:
                    prog[e].append(("dma", nd["fns"][0][0], self.dsem[nd["key"]][0]))
            if e == "sp":
                for k, v in self.dsem.items():
                    need("dma", k, v[1])
                for src in self.names:
                    tot = max([0] + [cnt[i] for i in order[src] if nodes[i]["kind"] == "op"])
                    if src != "sp":
                        need("eng", src, tot)

        def replay(name, eng):
            for item in prog[name]:
                if item[0] == "wait":
                    _, kind, src, val = item
                    s = self.sem[src] if kind == "eng" else self.dsem[src][0]
                    eng.wait_ge(s, val)
                elif item[0] == "op":
                    ins = item[1](eng)
                    if item[2]:
                        ins.then_inc(self.sem[name], 1)
                else:
                    item[1](eng).then_inc(item[2], 16)

        with nc.Block() as block:

            @block.tensor
            def _(e):
                replay("pe", e)

            @block.scalar
            def _(e):
                replay("act", e)

            @block.vector
            def _(e):
                replay("dve", e)

            @block.gpsimd
            def _(e):
                replay("pool", e)

            @block.sync
            def _(e):
                replay("sp", e)


def _consts():
    g = 1.0 - 2.0 ** (-5.0 - np.arange(NH, dtype=np.float64))
    p = np.arange(128, dtype=np.float64)
    c16 = {}
    c32 = {}
    c16["ident"] = np.eye(128)
    kk = np.arange(128)[:, None]
    qq = np.arange(128)[None, :]
    c16["trineg"] = np.where(kk <= qq, 0.0, NEG)
    wins = (2, 4, 8, 16)
    A0 = np.zeros((128, 4, 128))
    A1 = np.zeros((128, 4, 128))
    AP = np.zeros((128, 4, 128))
    for gi, w in enumerate(wins):
        for t in range(128):
            for s in range(t - w + 1, t + 1):
                if s >= 0:
                    A0[s, gi, t] += 1.0 / min(t + 1, w)
                    A1[s, gi, t] += 1.0 / w
                else:
                    AP[128 + s, gi, t] += 1.0 / w
            A0[t, gi, t] -= 1.0
            A1[t, gi, t] -= 1.0
    c16["A0"] = A0.reshape(128, 512)
    c16["A1"] = A1.reshape(128, 512)
    c16["AP"] = AP.reshape(128, 512)
    Ab = np.zeros((128, 4, 16))
    Ac = np.zeros((128, 4, 16))
    for gi, w in enumerate(wins):
        for b in range(4):
            for q in range(4):
                lo = 15 + q - w + 1
                for idx in range(lo, 15 + q + 1):
                    if idx < 15:
                        Ab[15 * b + idx, gi, 4 * b + q] += 1.0 / w
                    else:
                        Ac[4 * b + idx - 15, gi, 4 * b + q] += 1.0 / w
                Ac[4 * b + q, gi, 4 * b + q] -= 1.0
    c16["Ab"] = Ab.reshape(128, 64)
    c16["Ac"] = Ac.reshape(128, 64)
    blk = np.zeros((128, 2048))
    for c in range(8):
        blk[c, c * 256:(c + 1) * 256] = 1.0
    blkind = blk[:8].astype(ml_dtypes.bfloat16)
    c16["ones"] = np.ones((128, 128))
    dm = np.zeros((128, 390))
    for h in range(6):
        dm[16 * h:16 * h + 16, 65 * h:65 * h + 65] = 1.0
    c16["dmask"] = dm
    c32["mask01T"] = (kk <= qq).astype(np.float64)
    gq = g[None, :] ** p[:, None]
    gk = g[None, :] ** (-p[:, None]) * (HD ** -0.5)
    c32["gqk"] = np.concatenate([gq, gk], axis=1)
    dec = np.zeros((128, 30))
    dec[:, 0:6] = g ** 128
    dec[:, 6:12] = g ** 127
    dec[:, 12:18] = g
    dec[:, 18:24] = g ** 4
    dec[:, 24:30] = g ** 3
    c32["dec"] = dec
    ps = (np.arange(128) % 4).astype(np.float64)
    gqs = g[None, :] ** ps[:, None]
    gks = g[None, :] ** (-ps[:, None]) * (HD ** -0.5)
    c32["gqk_s"] = np.concatenate([gqs, gks], axis=1)
    j = np.arange(16)[:, None]
    i = np.arange(16)[None, :]
    same = (j // 4) == (i // 4)
    m01s = np.zeros((128, 16))
    m01s[:16] = (same & (i >= j)).astype(np.float64)
    c32["mask01s"] = m01s
    om = np.full((128, 16), NEG)
    om[:16] = np.where(same & (j <= i), 0.0, NEG)
    c32["ownmask"] = om
    bm = np.zeros((128, 4, 16))
    for b in range(4):
        bm[:, b, 4 * b:4 * b + 4] = 1.0
    c32["bmask"] = bm.reshape(128, 64)
    rm = np.zeros((128, 4))
    for b in range(4):
        rm[4 * b:4 * b + 4, b] = 1.0
    c32["rowmask"] = rm
    id24 = np.zeros((128, 24))
    id24[:24] = np.eye(24)
    c32["ident24"] = id24
    c32["ones"] = np.ones((128, 8))
    sel = np.zeros((128, 16))
    for r in range(96):
        sel[r, r % 16] = 1.0
    c32["sel96"] = sel
    c32["iota2"] = np.arange(128, dtype=np.float64)[:, None] + np.array([0.0, NPOOL * 128.0])[None, :]
    half = HD // 2
    inv = 1.0 / (10000.0 ** (np.arange(half, dtype=np.float32) / half))

    def tab(pos):
        ang = (pos.astype(np.float32)[:, None] * inv[None, :]).astype(np.float32)
        cos = np.cos(ang).astype(np.float32)
        sin = np.sin(ang).astype(np.float32)
        return np.concatenate([cos, cos, -sin, sin], axis=1).astype(np.float32)

    rope_p = tab(np.arange(SEQ))
    rs = np.zeros((128, 128), np.float32)
    rs[:16] = tab(8192 + (np.arange(16) % 4))
    c32["rope_s"] = rs

    def pack(dct, dt):
        offs = {}
        cols = []
        o = 0
        for k, v in dct.items():
            offs[k] = (o, v.shape[1])
            cols.append(v)
            o += v.shape[1]
        return offs, np.ascontiguousarray(np.concatenate(cols, axis=1)).astype(dt)

    o16, a16 = pack(c16, ml_dtypes.bfloat16)
    o32, a32 = pack(c32, np.float32)
    return o16, a16, o32, a32, (rope_p, blkind)


def build_nc(o16, n16, o32, n32):
    nc = bass.Bass("TRN2", target_bir_lowering=False)
    dt_in = lambda name, shape, dt=F32: nc.dram_tensor(name, shape, dt, kind="ExternalInput").ap()
    dt_out = lambda name, shape, dt=F32: nc.dram_tensor(name, shape, dt, kind="ExternalOutput").ap()
    xp = dt_in("xp", [SEQ, D])
    xs_d = dt_in("xs", [16, D])
    ck = dt_in("ck", [DEPTH, NPOOL, 128, 384])
    cv = dt_in("cv", [DEPTH, NPOOL, 128, 384])
    sret = dt_in("sret", [DEPTH, 4, NH, HD, HD])
    spool = dt_in("spool", [DEPTH, 60, 256])
    ptab = dt_in("ptab", [1, 256], I32)
    norm_w = dt_in("norm_w", [DEPTH, D])
    w_in = dt_in("w_in", [DEPTH, D, DIN])
    w_out = dt_in("w_out", [DEPTH, D, D])
    gn_w = dt_in("gn_w", [DEPTH, 384])
    qn_w = dt_in("qn_w", [DEPTH, HD])
    kn_w = dt_in("kn_w", [DEPTH, HD])
    pool_w = dt_in("pool_w", [DEPTH, 4, HD, HD])
    pool_b = dt_in("pool_b", [DEPTH, 4, HD])
    pool_s = dt_in("pool_s", [DEPTH, 256])
    c16d = dt_in("c16", [128, n16], BF16)
    c32d = dt_in("c32", [128, n32])
    roped = dt_in("rope_p", [SEQ, 128])
    blkd = dt_in("blkind", [8, SEQ], BF16)

    yp = dt_out("yp", [SEQ, D])
    ys = dt_out("ys", [16, D])
    kp = dt_out("kp", [DEPTH, SEQ, 384])
    vp = dt_out("vp", [DEPTH, SEQ, 384])
    ks = dt_out("ks", [DEPTH, 16, 384])
    vs = dt_out("vs", [DEPTH, 16, 384])
    rp = dt_out("rp", [DEPTH, NH, HD, HD])
    rs_o = dt_out("rs", [DEPTH, 4, NH, HD, HD])
    pp = dt_out("pp", [DEPTH, 15, 256])
    pso = dt_out("pso", [DEPTH, 4, 15, 256])
    y1s = nc.dram_tensor("y1s", [SEQ, D], F32, kind="Internal").ap()
    xs1 = nc.dram_tensor("xs1", [16, D], F32, kind="Internal").ap()

    with ExitStack() as st:
        S = Sched(nc, st)

        def sbt(stack, name, shape, dt):
            return stack.enter_context(nc.sbuf_tensor(name, shape, dt))

        PB = [st.enter_context(nc.psum_tensor("pb%d" % i, [128, 512], F32)) for i in range(8)]

        def pbf(i):
            return PB[i][:].bitcast(BF16)

        c16 = sbt(st, "c16s", [128, n16], BF16)
        c32 = sbt(st, "c32s", [128, n32], F32)
        win = sbt(st, "win", [128, 8, DIN], BF16)
        wout = sbt(st, "wout", [128, 8, D], BF16)
        nwb = sbt(st, "nwb", [128, D], F32)
        gnb = sbt(st, "gnb", [128, 384], F32)
        qkwb = sbt(st, "qkwb", [128, 2, HD], F32)
        psb = sbt(st, "psb", [128, 256], F32)
        pwa = sbt(st, "pwa", [65, 4, HD], BF16)

        def C16(name, rows=128):
            o, n = o16[name]
            return c16[0:rows, o:o + n]

        def C32(name, rows=128):
            o, n = o32[name]
            return c32[0:rows, o:o + n]

        S.dma("sp", lambda e: e.dma_start(out=c16[:], in_=c16d), writes=["c16"])
        S.dma("sp", lambda e: e.dma_start(out=c32[:], in_=c32d), writes=["c32"])

        ck_flat = ck.rearrange("l n p d -> (l n p) d")
        cv_flat = cv.rearrange("l n p d -> (l n p) d")

        def load_weights(l):
            ws = ExitStack()
            stg = [sbt(ws, "wstg%d_%d" % (l, i), [128, DIN], F32) for i in range(3)]
            stgo = [sbt(ws, "wsto%d_%d" % (l, i), [128, D], F32) for i in range(2)]
            segs_by_eng = (("dve", WIN_SEGS[0]), ("act", WIN_SEGS[2]), ("pool", WIN_SEGS[1]), ("pool", WIN_SEGS[3]),
                           ("pool", WIN_SEGS[4]), ("dve", WIN_SEGS[5]))
            for kc in range(8):
                bi = kc % 3
                S.dma("sp", lambda e, kc=kc, bi=bi: e.dma_start(out=stg[bi][:], in_=w_in[l, kc * 128:(kc + 1) * 128, :]),
                      writes=["wstg%d" % bi], lat=8.0)
                for eng, (so, sw, do) in segs_by_eng:
                    if eng == "act":
                        S.op("act", lambda e, kc=kc, bi=bi, so=so, sw=sw, do=do: e.copy(out=win[:, kc, do:do + sw], in_=stg[bi][:, so:so + sw]),
                             reads=["wstg%d" % bi], writes=["win_%d_%d" % (kc, do)], c=1.2)
                    else:
                        S.op(eng, lambda e, kc=kc, bi=bi, so=so, sw=sw, do=do: e.tensor_copy(out=win[:, kc, do:do + sw], in_=stg[bi][:, so:so + sw]),
                             reads=["wstg%d" % bi], writes=["win_%d_%d" % (kc, do)], c=(1.3 if eng == "dve" else 1.0))
            for kc in range(8):
                bi = kc % 2
                S.dma("sp", lambda e, kc=kc, bi=bi: e.dma_start(out=stgo[bi][:], in_=w_out[l, kc * 128:(kc + 1) * 128, :]),
                      writes=["wsto%d" % bi], lat=4.0)
                eng = ("dve", "act")[kc % 2]
                if eng == "act":
                    S.op("act", lambda e, kc=kc, bi=bi: e.copy(out=wout[:, kc, :], in_=stgo[bi][:]), reads=["wsto%d" % bi], writes=["wout_%d" % kc], c=1.0)
                else:
                    S.op("dve", lambda e, kc=kc, bi=bi: e.tensor_copy(out=wout[:, kc, :], in_=stgo[bi][:]), reads=["wsto%d" % bi], writes=["wout_%d" % kc], c=1.0)
            load_small(l)
            S.barrier()
            ws.close()

        def load_small(l):
            S.dma("sp", lambda e: e.dma_start(out=nwb[:], in_=norm_w[l:l + 1, :].to_broadcast([128, D])), writes=["nwb"])
            S.dma("sp", lambda e: e.dma_start(out=gnb[:], in_=gn_w[l:l + 1, :].to_broadcast([128, 384])), writes=["gnb"])
            S.dma("sp", lambda e: e.dma_start(out=qkwb[:, 0, :], in_=qn_w[l:l + 1, :].to_broadcast([128, HD])), writes=["qkwb"], key="dqkw")
            S.dma("sp", lambda e: e.dma_start(out=qkwb[:, 1, :], in_=kn_w[l:l + 1, :].to_broadcast([128, HD])), writes=["qkwb"], key="dqkw")
            S.dma("sp", lambda e: e.dma_start(out=psb[:], in_=pool_s[l:l + 1, :].to_broadcast([128, 256])), writes=["psb"])
            S.dma("pool", lambda e: e.dma_start(out=pwa[0:64, :, :], in_=pool_w[l].rearrange("g c d -> c g d")), writes=["pwa"], key="dpwa")
            S.dma("pool", lambda e: e.dma_start(out=pwa[64:65, :, :], in_=pool_b[l:l + 1, :, :]), writes=["pwa"], key="dpwa")

        def tt(eng, out, in0, in1, op, reads, writes):
            S.op(eng, lambda e: e.tensor_tensor(out=out, in0=in0, in1=in1, op=op), reads=reads, writes=writes)

        def ts(eng, out, in0, s1, s2, op0, op1, reads, writes):
            if op1 is None:
                S.op(eng, lambda e: e.tensor_scalar(out=out, in0=in0, scalar1=s1, scalar2=None, op0=op0), reads=reads, writes=writes)
            else:
                S.op(eng, lambda e: e.tensor_scalar(out=out, in0=in0, scalar1=s1, scalar2=s2, op0=op0, op1=op1), reads=reads, writes=writes)

        def rsq(out, in_, scale, bias, reads, writes):
            S.op("act", lambda e: e.activation(out=out, in_=in_, func=AF.Ln, scale=scale, bias=bias), reads=reads, writes=writes)
            S.op("act", lambda e: e.activation(out=out, in_=out, func=AF.Exp, scale=-0.5), reads=writes, writes=writes)

        def silu_gates(G, zs, tg):
            zg = zs[:, O_RG:O_RG + 1024]
            S.op("act", lambda e: e.activation(out=G[:], in_=zg, func=AF.Exp, scale=-1.0), reads=[tg + "zs"], writes=[tg + "G"])
            S.op("act", lambda e: e.activation(out=G[:], in_=G[:], func=AF.Ln, bias=1.0), reads=[tg + "G"], writes=[tg + "G"])
            S.op("act", lambda e: e.activation(out=G[:], in_=G[:], func=AF.Exp, scale=-1.0), reads=[tg + "G"], writes=[tg + "G"])
            tt("dve", G[:], G[:], zg, ALU.mult, [tg + "G", tg + "zs"], [tg + "G"])

        def rope(P, x768, cs, t1, t2, out768, tag, rd, okey=None):
            xv = x768.rearrange("p (h t d) -> p h t d", h=12, t=2)
            t1v = t1.rearrange("p (h t d) -> p h t d", h=12, t=2)
            t2v = t2.rearrange("p (h t d) -> p h t d", h=12, t=2)
            cosb = cs[:, 0:64].rearrange("p (t d) -> p t d", t=2).unsqueeze(1).to_broadcast([P, 12, 2, 32])
            nsin = cs[:, 64:96].unsqueeze(1).to_broadcast([P, 12, 32])
            psin = cs[:, 96:128].unsqueeze(1).to_broadcast([P, 12, 32])
            tk = tag[:-1]
            tt("dve", t1v, xv, cosb, ALU.mult, rd, [tk + "t1"])
            tt("dve", t2v[:, :, 0, :], xv[:, :, 1, :], nsin, ALU.mult, rd, [tk + "t2a"])
            tt("dve", t2v[:, :, 1, :], xv[:, :, 0, :], psin, ALU.mult, rd, [tk + "t2b"])
            tt("dve", out768, t1, t2, ALU.add, [tk + "t1", tk + "t2a", tk + "t2b"], [okey or (tag + "rot")])

        def sample_layer(l, kst):
            P = 16
            tg0 = "s%d_" % l
            kb_ = lambda name, shape, dt: sbt(kst, "k%d_" % l + name, shape, dt)
            mix = kb_("mix", [P, D], BF16)
            Gm = kb_("Gm", [P, 384], F32)
            vna = kb_("vna", [P, 6, 65], BF16)
            ptown = kb_("ptown", [P, 6, P], BF16)
            acc = kb_("acc", [P, 390], F32)
            Qblk = kb_("Qblk", [128, 3, 4, 2, 4], BF16)
            idxt = kb_("idxt", [128, 256], I32)
            NKB = 4
            kpg = [kb_("kpg%d" % i, [128, 384], BF16) for i in range(NKB)]
            kT = [kb_("kT%d" % i, [128, 3, 128], BF16) for i in range(2)]
            vau = [kb_("vau%d" % i, [128, 6, 65], BF16) for i in range(2)]
            Sall = kb_("Sall", [128, 64, 24], F32)
            PTp = kb_("PTp", [128, 16, 6, P], BF16)
            gate = kb_("gate", [24, 32], F32)
            m8 = kb_("m8", [24, 8], F32)
            negm = kb_("negm", [24, 32], F32)
            Dm = kb_("Dm", [24, 32, 24], BF16)
            ss = ExitStack()
            sb = lambda name, shape, dt: sbt(ss, "s%d_" % l + name, shape, dt)
            pts = sb("pts", [128, 256], I32)
            xss = sb("xss", [P, D], F32)
            xsrc = xs_d if l == 0 else xs1
            S.dma("sp", lambda e: e.dma_start(out=xss[:], in_=xsrc), reads=(["xs1"] if l > 0 else []), writes=[tg0 + "xss"])
            hb = sb("hb", [P, D], BF16)
            junk = sb("junk", [P, D], BF16)
            ssq = sb("ssq", [P, 4], F32)
            hT = sb("hT", [128, 8, P], BF16)
            zs = sb("zs", [P, DIN], F32)
            t1 = sb("t1", [P, 768], F32)
            t2 = sb("t2", [P, 768], F32)
            rot = sb("rot", [P, 768], F32)
            rqk = sb("rqk", [P, 768], BF16)
            rqkT = sb("rqkT", [64, 12, P], BF16)
            rvb = sb("rvb", [P, 384], BF16)
            kpad = sb("kpad", [P, 4, 384], BF16)
            qpad = sb("qpad", [64, 6, 4, P], BF16)
            S0 = sb("S0", [64, 4, NH, HD], F32)
            gS0 = sb("gS0", [64, 4, NH, HD], BF16)
            Sn = sb("Sn", [64, 4, NH, HD], F32)
            inT = sb("inT", [P, 6, P], BF16)
            osb = sb("osb", [P, 384], F32)
            osq = sb("osq", [P, 384], F32)
            st1 = sb("st1", [P, 6], F32)
            st2 = sb("st2", [P, 6], F32)
            st3 = sb("st3", [P, 6], F32)
            G = sb("G", [P, D], F32)
            mrot = sb("mrot", [P, 768], F32)
            mnorm = sb("mnorm", [P, 768], F32)
            mss = sb("mss", [P, 12], F32)
            mqkb = sb("mqkb", [P, 768], BF16)
            mqkT = sb("mqkT", [64, 12, P], BF16)
            qTp = sb("qTp", [128, 3, P], BF16)
            sown = sb("sown", [P, 6, P], F32)
            bufs = sb("bufs", [60, 256], F32)
            bufb = sb("bufb", [60, 256], BF16)
            ub = sb("ub", [P, 256], BF16)
            pT = sb("pT", [65, 4, P], BF16)
            pos = sb("pos", [P, 256], F32)
            tg = "s%d_" % l
            S.dma("sp", lambda e: e.dma_start(out=pts[:], in_=ptab.to_broadcast([128, 256])), writes=[tg + "pts"])
            S.op("dve", lambda e: e.tensor_scalar(out=idxt[:], in0=pts[:], scalar1=128.0, scalar2=C32("iota2")[:, l:l + 1], op0=ALU.mult, op1=ALU.add),
                 reads=[tg + "pts", "c32"], writes=[tg + "idxt"])
            cs = C32("rope_s", P)

            S.dma("sp", lambda e: e.dma_start(out=S0[:], in_=sret[l].rearrange("b h d e -> d b h e")), writes=[tg + "S0"])
            S.dma("sp", lambda e: e.dma_start(out=bufs[:], in_=spool[l]), writes=[tg + "bufs"])
            S.op("pool", lambda e: e.tensor_copy(out=bufb[:], in_=bufs[:]), reads=[tg + "bufs"], writes=[tg + "bufb"])
            for b in range(4):
                S.dma("sp", lambda e, b=b: e.dma_start(out=pso[l, b, 0:11, :], in_=bufs[15 * b + 4:15 * b + 15, :]),
                      reads=[tg + "bufs"], key="o_pso")
            S.op("act", lambda e: e.activation(out=junk[:], in_=xss[:], func=AF.Square, accum_out=ssq[:, 0:1]),
                 reads=[tg + "xss"], writes=[tg + "ssq", tg + "junk"])
            rsq(ssq[:, 2:3], ssq[:, 0:1], 1.0 / D, EPS, [tg + "ssq"], [tg + "rstd"])
            rstd = ssq[:, 2:3]
            tt("dve", hb[:], xss[:], nwb[0:P, :], ALU.mult, [tg + "xss", "nwb"], [tg + "hb"])
            for kc in range(8):
                S.op("pe", lambda e, kc=kc: e.transpose(out=pbf(2)[:, kc * P:(kc + 1) * P], in_=hb[:, kc * 128:(kc + 1) * 128], identity=C16("ident", P)[:, 0:P]),
                     reads=[tg + "hb", "c16"], writes=["pb2"], mark=(kc == 7))
            S.op("dve", lambda e: e.tensor_copy(out=hT[:].rearrange("p k t -> p (k t)"), in_=pbf(2)[:, 0:8 * P]), reads=["pb2"], writes=[tg + "hT"])
            for n in range(7):
                bk = n % 2
                for kc in range(8):
                    S.op("pe", lambda e, kc=kc, n=n, bk=bk: e.matmul(PB[bk][0:P, :], lhsT=hT[:, kc, :], rhs=win[:, kc, n * 512:(n + 1) * 512], start=(kc == 0), stop=(kc == 7)),
                         reads=[tg + "hT", "win"], writes=["pb%d" % bk], mark=(kc == 7))
                S.op("act", lambda e, n=n, bk=bk: e.activation(out=zs[:, n * 512:(n + 1) * 512], in_=PB[bk][0:P, :], func=AF.Copy, scale=rstd),
                     reads=["pb%d" % bk, tg + "rstd"], writes=[tg + "zs"])
            S.dma("sp", lambda e: e.dma_start(out=vs[l], in_=zs[:, O_MV:O_MV + 384]), reads=[tg + "zs"], key="o_vs")
            S.dma("sp", lambda e: e.dma_start(out=pso[l, :, 11:15, :].rearrange("b q c -> (b q) c") if False else pso[l, 0, 11:15, :], in_=zs[0:4, O_PU:O_PU + 256]), reads=[tg + "zs"], key="o_pso")
            for b in range(1, 4):
                S.dma("sp", lambda e, b=b: e.dma_start(out=pso[l, b, 11:15, :], in_=zs[4 * b:4 * b + 4, O_PU:O_PU + 256]), reads=[tg + "zs"], key="o_pso")
            silu_gates(G, zs, tg)
            S.op("act", lambda e: e.copy(out=Gm[:], in_=G[:, 384:768]), reads=[tg + "G"], writes=[tg + "Gm"])
            rope(P, zs[:, 0:768], cs, t1[:], t2[:], rot[:], tg + "r", [tg + "zs", "c32"])
            tt("dve", rqk[:].rearrange("p (h d) -> p h d", h=12), rot[:].rearrange("p (h d) -> p h d", h=12),
               C32("gqk_s", P).unsqueeze(2).to_broadcast([P, 12, 64]), ALU.mult, [tg + "rrot", "c32"], [tg + "rqk"])
            S.op("pool", lambda e: e.tensor_copy(out=rvb[:], in_=zs[:, O_RV:O_RV + 384]), reads=[tg + "zs"], writes=[tg + "rvb"])
            for h in range(12):
                S.op("pe", lambda e, h=h: e.transpose(out=pbf(3)[0:64, h * P:(h + 1) * P], in_=rqk[:, h * 64:(h + 1) * 64], identity=C16("ident", P)[:, 0:P]),
                     reads=[tg + "rqk", "c16"], writes=["pb3"], mark=(h == 11))
            S.op("dve", lambda e: e.tensor_copy(out=rqkT[:].rearrange("p h t -> p (h t)"), in_=pbf(3)[0:64, 0:12 * P]), reads=["pb3"], writes=[tg + "rqkT"])
            tt("dve", qpad[:], rqkT[:, 0:6, :].unsqueeze(2).to_broadcast([64, 6, 4, P]),
               C32("bmask", 64).rearrange("p (b t) -> p b t", b=4).unsqueeze(1).to_broadcast([64, 6, 4, P]), ALU.mult,
               [tg + "rqkT", "c32"], [tg + "qpad"])
            tt("dve", kpad[:], rqk[:, 384:768].unsqueeze(1).to_broadcast([P, 4, 384]),
               C32("rowmask", P).unsqueeze(2).to_broadcast([P, 4, 384]), ALU.mult, [tg + "rqk", "c32"], [tg + "kpad"])
            tt("dve", gS0[:], S0[:], C32("dec", 64)[:, 12:18].unsqueeze(1).unsqueeze(3).to_broadcast([64, 4, NH, HD]), ALU.mult,
               [tg + "S0", "c32"], [tg + "gS0"])
            for h in range(6):
                S.op("pe", lambda e, h=h: e.matmul(PB[4][0:P, h * P:(h + 1) * P], lhsT=rqkT[:, 6 + h, :], rhs=rqkT[:, h, :], start=True, stop=True),
                     reads=[tg + "rqkT"], writes=["pb4"], mark=(h == 5))
            tt("dve", inT[:], PB[4][0:P, 0:6 * P].rearrange("p (h t) -> p h t", h=6),
               C32("mask01s", P).unsqueeze(1).to_broadcast([P, 6, P]), ALU.mult, ["pb4", "c32"], [tg + "inT"])
            for h in range(6):
                S.op("pe", lambda e, h=h: e.matmul(PB[5][0:P, h * 64:(h + 1) * 64], lhsT=inT[:, h, :], rhs=rvb[:, h * 64:(h + 1) * 64], start=True, stop=False),
                     reads=[tg + "inT", tg + "rvb"], writes=["pb5"], mark=False)
                for b in range(4):
                    S.op("pe", lambda e, h=h, b=b: e.matmul(PB[5][0:P, h * 64:(h + 1) * 64], lhsT=qpad[:, h, b, :], rhs=gS0[:, b, h, :], start=False, stop=(b == 3)),
                         reads=[tg + "qpad", tg + "gS0"], writes=["pb5"], mark=(b == 3 and h == 5))
            for b in range(4):
                for h in range(6):
                    S.op("pe", lambda e, h=h, b=b: e.matmul(PB[4][0:64, h * 64:(h + 1) * 64], lhsT=kpad[:, b, h * 64:(h + 1) * 64], rhs=rvb[:, h * 64:(h + 1) * 64], start=True, stop=True),
                         reads=[tg + "kpad", tg + "rvb"], writes=["pb4"], mark=(h == 5))
                tt("dve", Sn[:, b, :, :], PB[4][0:64, 0:384].rearrange("p (h e) -> p h e", h=6),
                   C32("dec", 64)[:, 24:30].unsqueeze(2).to_broadcast([64, NH, HD]), ALU.mult, ["pb4", "c32"], [tg + "Sn"])
                tt("pool", S0[:, b, :, :], S0[:, b, :, :], C32("dec", 64)[:, 18:24].unsqueeze(2).to_broadcast([64, NH, HD]), ALU.mult,
                   [tg + "gS0", "c32"], [tg + "S0"])
                tt("dve", Sn[:, b, :, :], Sn[:, b, :, :], S0[:, b, :, :], ALU.add, [tg + "Sn", tg + "S0"], [tg + "Sn"])
            S.dma("sp", lambda e: e.dma_start(out=rs_o[l].rearrange("b h d e -> d b h e"), in_=Sn[:]), reads=[tg + "Sn"], key="o_rs")
            group_norm(P, PB[5][0:P, 0:384], "pb5", osb, osq, st1, st2, st3, G[:, 0:384], mix[:, 0:384], tg)
            S.op("pool", lambda e: e.tensor_copy(out=ub[:], in_=zs[:, O_PU:O_PU + 256]), reads=[tg + "zs"], writes=[tg + "ub"])
            for g in range(4):
                S.op("pe", lambda e, g=g: e.matmul(PB[4][0:64, g * P:(g + 1) * P], lhsT=bufb[:, g * 64:(g + 1) * 64], rhs=C16("Ab", 60)[:, g * 16:(g + 1) * 16], start=True, stop=False),
                     reads=[tg + "bufb", "c16"], writes=["pb4"], mark=False)
                S.op("pe", lambda e, g=g: e.matmul(PB[4][0:64, g * P:(g + 1) * P], lhsT=ub[:, g * 64:(g + 1) * 64], rhs=C16("Ac", P)[:, g * 16:(g + 1) * 16], start=False, stop=True),
                     reads=[tg + "ub", "c16"], writes=["pb4"], mark=(g == 3))
            S.op("pool", lambda e: e.memset(pT[64:65, :, :], 1.0), writes=[tg + "pT1"])
            S.op("dve", lambda e: e.tensor_copy(out=pT[0:64, :, :].rearrange("p g t -> p (g t)"), in_=PB[4][0:64, 0:4 * P]), reads=["pb4"], writes=[tg + "pT"])
            for g in range(4):
                S.op("pe", lambda e, g=g: e.matmul(PB[5][0:P, g * 64:(g + 1) * 64], lhsT=pT[0:65, g, :], rhs=pwa[0:65, g, :], start=True, stop=True),
                     reads=[tg + "pT", tg + "pT1", "pwa"], writes=["pb5"], mark=(g == 3))
            tt("dve", pos[:], PB[5][0:P, 0:256], psb[0:P, :], ALU.mult, ["pb5", "psb"], [tg + "pos"])
            tt("dve", mix[:, 768:1024], pos[:], G[:, 768:1024], ALU.mult, [tg + "pos", tg + "G"], [tg + "mixp"])
            qk_norm(P, zs[:, O_MQ:O_MQ + 768], tg + "zs", mnorm, osq_big=t1, mss=mss, tag=tg)
            rope(P, mnorm[:], cs, t1[:], t2[:], mrot[:], tg + "m", [tg + "mnorm", "c32"])
            S.dma("sp", lambda e: e.dma_start(out=ks[l], in_=mrot[:, 384:768]), reads=[tg + "mrot"], key="o_ks")
            S.op("pool", lambda e: e.tensor_copy(out=mqkb[:], in_=mrot[:]), reads=[tg + "mrot"], writes=[tg + "mqkb"])
            S.op("pool", lambda e: e.memset(vna[:, :, 64:65], 1.0), writes=[tg + "vna1"])
            S.op("pool", lambda e: e.tensor_copy(out=vna[:, :, 0:64], in_=zs[:, O_MV:O_MV + 384].rearrange("p (h d) -> p h d", h=6)), reads=[tg + "zs"], writes=[tg + "vna"])
            for h in range(12):
                S.op("pe", lambda e, h=h: e.transpose(out=pbf(3)[0:64, h * P:(h + 1) * P], in_=mqkb[:, h * 64:(h + 1) * 64], identity=C16("ident", P)[:, 0:P]),
                     reads=[tg + "mqkb", "c16"], writes=["pb3"], mark=(h == 11))
            S.op("dve", lambda e: e.tensor_copy(out=mqkT[:].rearrange("p h t -> p (h t)"), in_=pbf(3)[0:64, 0:12 * P]), reads=["pb3"], writes=[tg + "mqkT"])
            for c in range(3):
                S.op("pe", lambda e, c=c: e.transpose(out=pbf(2)[:, c * P:(c + 1) * P], in_=mqkb[:, c * 128:(c + 1) * 128], identity=C16("ident", P)[:, 0:P]),
                     reads=[tg + "mqkb", "c16"], writes=["pb2"], mark=(c == 2))
            S.op("dve", lambda e: e.tensor_copy(out=qTp[:].rearrange("p c t -> p (c t)"), in_=pbf(2)[:, 0:3 * P]), reads=["pb2"], writes=[tg + "qTp"])
            S.op("pool", lambda e: e.memset(Qblk[:].rearrange("p c b s q -> p (c b s q)"), 0.0), writes=[tg + "Qblk"])
            for hp in range(2):
                S.op("dve", lambda e, hp=hp: e.tensor_copy(out=Qblk[hp * 64:(hp + 1) * 64, :, :, hp, :],
                                                          in_=qTp[hp * 64:(hp + 1) * 64, :, :].rearrange("p c (b q) -> p c b q", b=4)),
                     reads=[tg + "qTp", tg + "Qblk"], writes=[tg + "Qblk"])
            for h in range(6):
                S.op("pe", lambda e, h=h: e.matmul(PB[4][0:P, h * P:(h + 1) * P], lhsT=mqkT[:, 6 + h, :], rhs=mqkT[:, h, :], start=True, stop=True),
                     reads=[tg + "mqkT"], writes=["pb4"], mark=(h == 5))
            tt("dve", sown[:], PB[4][0:P, 0:6 * P].rearrange("p (h t) -> p h t", h=6),
               C32("ownmask", P).unsqueeze(1).to_broadcast([P, 6, P]), ALU.add, ["pb4", "c32"], [tg + "sown"])
            S.op("act", lambda e: e.activation(out=ptown[:], in_=sown[:], func=AF.Exp, scale=0.125), reads=[tg + "sown"], writes=[tg + "ptown"])

            S.barrier()
            ss.close()
            yield "pre"
            for i in range(2):
                S.op("pool", lambda e, i=i: e.memset(vau[i][:, :, 64:65], 1.0), writes=[tg + "vau1_%d" % i])
            kcnt = [0]

            def page_dma(cache, b, pg, slot):
                col = b * 64 + pg
                S.dma("pool", lambda e: e.indirect_dma_start(out=kpg[slot][:], out_offset=None, in_=cache,
                                                             in_offset=bass.IndirectOffsetOnAxis(ap=idxt[:, col:col + 1], axis=0)),
                      reads=[tg + "idxt"], writes=[tg + "kpg%d" % slot])

            for b in range(4):
                for pg in range(NPAGES):
                    n = kcnt[0]
                    kcnt[0] += 1
                    slot = n % NKB
                    s2 = n % 2
                    page_dma(ck_flat, b, pg, slot)
                    for c in range(3):
                        S.op("pe", lambda e, c=c, slot=slot: e.transpose(out=pbf(7)[:, c * 128:(c + 1) * 128], in_=kpg[slot][:, c * 128:(c + 1) * 128], identity=C16("ident")),
                             reads=[tg + "kpg%d" % slot, "c16"], writes=["pb7"], mark=(c == 2))
                    if pg % 2 == 0:
                        S.op("dve", lambda e, s2=s2: e.tensor_copy(out=kT[s2][:].rearrange("p c k -> p (c k)"), in_=pbf(7)[:, 0:384]),
                             reads=["pb7"], writes=[tg + "kT%d" % s2])
                    else:
                        S.op("act", lambda e, s2=s2: e.copy(out=kT[s2][:].rearrange("p c k -> p (c k)"), in_=pbf(7)[:, 0:384]),
                             reads=["pb7"], writes=[tg + "kT%d" % s2])
                    pl = pg % 8
                    for c in range(3):
                        S.op("pe", lambda e, c=c, s2=s2, pl=pl, b=b: e.matmul(PB[7][:, 256 + pl * 24 + c * 8:256 + pl * 24 + c * 8 + 8], lhsT=kT[s2][:, c, :],
                                                                            rhs=Qblk[:, c, b, :, :].rearrange("p s q -> p (s q)"), start=True, stop=True),
                             reads=[tg + "kT%d" % s2, tg + "Qblk"], writes=["pb7"], mark=(c == 2))
                    if pl == 7:
                        p0 = pg - 7
                        S.op("act", lambda e, p0=p0: e.copy(out=Sall[:, p0:p0 + 8, :].rearrange("p a b -> p (a b)"), in_=PB[7][:, 256:448]),
                             reads=["pb7"], writes=[tg + "Sall"])
                    yield "page"
                for pg in range(NPAGES):
                    S.op("pe", lambda e, pg=pg: e.matmul(PB[7][0:24, 448 + pg // 2:448 + pg // 2 + 1], lhsT=Sall[:, pg, :], rhs=C32("ones")[:, 0:1], start=(pg % 2 == 0), stop=(pg % 2 == 1)),
                         reads=[tg + "Sall", "c32"], writes=["pb7"], mark=(pg == NPAGES - 1))
                S.op("dve", lambda e: e.tensor_copy(out=gate[:], in_=PB[7][0:24, 448:480]), reads=["pb7"], writes=[tg + "gate"])
                S.op("dve", lambda e: e.max(out=m8[:], in_=gate[:]), reads=[tg + "gate"], writes=[tg + "m8"])
                tt("dve", negm[:], gate[:], m8[:, 2:3].to_broadcast([24, 32]), ALU.is_ge, [tg + "gate", tg + "m8"], [tg + "negm"])
                ts("dve", negm[:], negm[:], -1.0, -NEG, ALU.add, ALU.mult, [tg + "negm"], [tg + "negm"])
                tt("dve", Dm[:], negm[:].unsqueeze(2).to_broadcast([24, 32, 24]),
                   C32("ident24", 24).unsqueeze(1).to_broadcast([24, 32, 24]), ALU.mult, [tg + "negm", "c32"], [tg + "Dm"])
                yield "gate"
                for hh in range(2):
                    S.op("pe", lambda e, hh=hh: e.matmul(PB[7][:, 0:384], lhsT=C16("ones", 24), rhs=Dm[:, hh * 16:(hh + 1) * 16, :].rearrange("p a b -> p (a b)"), start=True, stop=True),
                         reads=[tg + "Dm", "c16"], writes=["pb7"])
                    tt("dve", Sall[:, hh * 32:(hh + 1) * 32, :].rearrange("p (k t) c -> p k t c", t=2),
                       Sall[:, hh * 32:(hh + 1) * 32, :].rearrange("p (k t) c -> p k t c", t=2),
                       PB[7][:, 0:384].rearrange("p (k c) -> p k c", k=16).unsqueeze(2).to_broadcast([128, 16, 2, 24]), ALU.add,
                       [tg + "Sall", "pb7"], [tg + "Sall"])
                    yield "mask"
                S.op("dve", lambda e: e.memset(PTp[:].rearrange("p a h t -> p (a h t)"), 0.0), writes=[tg + "PTp"])
                for pg in range(NPAGES):
                    if pg % 16 == 0:
                        S.op("act", lambda e, b=b, pg=pg: e.activation(out=PTp[:, :, :, 4 * b:4 * b + 4], in_=Sall[:, pg:pg + 16, :].rearrange("p a (h q) -> p a h q", h=6), func=AF.Exp, scale=0.125),
                             reads=[tg + "Sall"], writes=[tg + "PTp"])
                    n = kcnt[0]
                    kcnt[0] += 1
                    slot = n % NKB
                    s2 = n % 2
                    page_dma(cv_flat, b, pg, slot)
                    S.op("act", lambda e, slot=slot, s2=s2: e.copy(out=vau[s2][:, :, 0:64], in_=kpg[slot][:].rearrange("p (h d) -> p h d", h=6)),
                         reads=[tg + "kpg%d" % slot], writes=[tg + "vau%d" % s2])
                    S.op("pe", lambda e, pg=pg, s2=s2: e.matmul(PB[7][0:96, 0:390], lhsT=PTp[:, pg % 16, :, :].rearrange("p h t -> p (h t)"),
                                                              rhs=vau[s2][:].rearrange("p h e -> p (h e)"), start=(pg == 0), stop=(pg == NPAGES - 1)),
                         reads=[tg + "PTp", tg + "vau%d" % s2, tg + "vau1_%d" % s2], writes=["pb7"], c=0.22)
                    yield "page"
                msk = Sall[0:96, 0:17, :].rearrange("p a b -> p (a b)")[:, 0:390]
                tt("dve", msk, PB[7][0:96, 0:390], C16("dmask", 96), ALU.mult, ["pb7", "c16"], [tg + "Sall"])
                S.op("pe", lambda e: e.matmul(PB[7][0:P, 0:390], lhsT=C32("sel96", 96), rhs=msk, start=True, stop=True),
                     reads=[tg + "Sall", "c32"], writes=["pb7"], c=0.8)
                if b == 0:
                    S.op("dve", lambda e: e.tensor_copy(out=acc[:], in_=PB[7][0:P, 0:390]), reads=["pb7"], writes=[tg + "acc"])
                else:
                    tt("dve", acc[:], acc[:], PB[7][0:P, 0:390], ALU.add, [tg + "acc", "pb7"], [tg + "acc"])
            yield "pages"
            ss = ExitStack()
            sb = lambda name, shape, dt: sbt(ss, "t%d_" % l + name, shape, dt)
            mixT = sb("mixT", [128, 8, P], BF16)
            xs2 = sb("xs2", [P, D], F32)
            S.dma("sp", lambda e: e.dma_start(out=xs2[:], in_=xsrc), writes=[tg + "xs2"])
            mo = sb("mo", [P, 6, 64], F32)
            rden = sb("rden", [P, 6], F32)
            for h in range(6):
                S.op("pe", lambda e, h=h: e.matmul(PB[3][0:P, h * 65:(h + 1) * 65], lhsT=ptown[:, h, :], rhs=vna[:, h, :], start=True, stop=True),
                     reads=[tg + "ptown", tg + "vna", tg + "vna1"], writes=["pb3"], mark=(h == 5))
            tt("dve", acc[:], acc[:], PB[3][0:P, 0:390], ALU.add, [tg + "acc", "pb3"], [tg + "acc"])
            pvv = acc[:].rearrange("p (h e) -> p h e", h=6)
            S.op("dve", lambda e: e.reciprocal(out=rden[:].unsqueeze(2), in_=pvv[:, :, 64:65]), reads=[tg + "acc"], writes=[tg + "rden"])
            tt("dve", mo[:], pvv[:, :, 0:64], rden[:].unsqueeze(2).to_broadcast([P, 6, 64]), ALU.mult, [tg + "acc", tg + "rden"], [tg + "mo"])
            tt("dve", mix[:, 384:768], mo[:].rearrange("p h e -> p (h e)"), Gm[:], ALU.mult, [tg + "mo", tg + "Gm"], [tg + "mixm"])
            for kc in range(8):
                S.op("pe", lambda e, kc=kc: e.transpose(out=pbf(2)[:, kc * P:(kc + 1) * P], in_=mix[:, kc * 128:(kc + 1) * 128], identity=C16("ident", P)[:, 0:P]),
                     reads=[tg + "mixr", tg + "mixm", tg + "mixp", "c16"], writes=["pb2"], mark=(kc == 7))
            S.op("dve", lambda e: e.tensor_copy(out=mixT[:].rearrange("p k t -> p (k t)"), in_=pbf(2)[:, 0:8 * P]), reads=["pb2"], writes=[tg + "mixT"])
            for n in range(2):
                for kc in range(8):
                    S.op("pe", lambda e, kc=kc, n=n: e.matmul(PB[n][0:P, :], lhsT=mixT[:, kc, :], rhs=wout[:, kc, n * 512:(n + 1) * 512], start=(kc == 0), stop=(kc == 7)),
                         reads=[tg + "mixT", "wout"], writes=["pb%d" % n], mark=(kc == 7))
                tt("dve", xs2[:, n * 512:(n + 1) * 512], PB[n][0:P, :], xs2[:, n * 512:(n + 1) * 512], ALU.add, ["pb%d" % n, tg + "xs2"], [tg + "xs2"])
            if l == DEPTH - 1:
                S.dma("sp", lambda e: e.dma_start(out=ys, in_=xs2[:]), reads=[tg + "xs2"], key="o_ys")
            else:
                S.dma("sp", lambda e: e.dma_start(out=xs1, in_=xs2[:]), reads=[tg + "xs2"], writes=["xs1"], key="dxs1")

            S.barrier()
            ss.close()
            yield "post"

        def group_norm(P, o_ps, o_key, osb, osq, st1, st2, st3, Gs, out_mix, tg, gkeys=None):
            S.op("act", lambda e: e.copy(out=osb[:], in_=o_ps), reads=[o_key], writes=[tg + "osb"])
            ov = osb[:].rearrange("p (h d) -> p h d", h=6)
            S.op("dve", lambda e: e.tensor_reduce(out=st1[:], in_=ov, axis=AX.X, op=ALU.add), reads=[tg + "osb"], writes=[tg + "st1"])
            ts("dve", st1[:], st1[:], 1.0 / 64, None, ALU.mult, None, [tg + "st1"], [tg + "st1"])
            tt("dve", ov, ov, st1[:].unsqueeze(2).to_broadcast([P, 6, 64]), ALU.subtract, [tg + "osb", tg + "st1"], [tg + "osb"])
            tt("dve", osq[:], osb[:], osb[:], ALU.mult, [tg + "osb"], [tg + "osq"])
            S.op("dve", lambda e: e.tensor_reduce(out=st2[:], in_=osq[:].rearrange("p (h d) -> p h d", h=6), axis=AX.X, op=ALU.add), reads=[tg + "osq"], writes=[tg + "st2"])
            rsq(st3[:], st2[:], 1.0 / 64, EPS, [tg + "st2"], [tg + "st3"])
            tt("dve", ov, ov, st3[:].unsqueeze(2).to_broadcast([P, 6, 64]), ALU.mult, [tg + "osb", tg + "st3"], [tg + "osb"])
            tt("dve", osb[:], osb[:], gnb[0:P, :], ALU.mult, [tg + "osb", "gnb"], [tg + "osb"])
            tt("dve", out_mix, osb[:], Gs, ALU.mult, [tg + "osb"] + (gkeys or [tg + "G"]), [tg + "mixr"])

        def qk_norm(P, x768, xkey, mnorm, osq_big, mss, tag):
            S.op("act", lambda e: e.activation(out=osq_big[:], in_=x768, func=AF.Square), reads=[xkey], writes=[tag + "t1"])
            S.op("dve", lambda e: e.tensor_reduce(out=mss[:], in_=osq_big[:].rearrange("p (h d) -> p h d", h=12), axis=AX.X, op=ALU.add),
                 reads=[tag + "t1"], writes=[tag + "mss"])
            rsq(mss[:], mss[:], 1.0 / 64, EPS, [tag + "mss"], [tag + "mss"])
            mn = x768 if mnorm is None else mnorm[:]
            mk_ = xkey if mnorm is None else tag + "mnorm"
            tt("dve", mn.rearrange("p (h d) -> p h d", h=12), x768.rearrange("p (h d) -> p h d", h=12),
               mss[:].unsqueeze(2).to_broadcast([P, 12, 64]), ALU.mult, [xkey, tag + "mss"], [mk_])
            tt("dve", mn.rearrange("p (s h d) -> p s h d", s=2, h=6), mn.rearrange("p (s h d) -> p s h d", s=2, h=6),
               qkwb[0:P, :, :].unsqueeze(2).to_broadcast([P, 2, 6, 64]), ALU.mult, [mk_, "qkwb"], [mk_])

        def prompt_layer(l, ps_, pump):
            P = 128
            sb = lambda name, shape, dt: sbt(ps_, "p%d_" % l + name, shape, dt)
            xt = [sb("xt%d" % i, [P, D], F32) for i in range(2)]
            cst = [sb("cs%d" % i, [P, 128], F32) for i in range(2)]
            hb = sb("hb", [P, D], BF16)
            ssq = sb("ssq", [P, 4], F32)
            hT = sb("hT", [128, 8, P], BF16)
            zs = sb("zs", [P, O_RG], F32)
            t1 = sb("t1", [P, 768], F32)
            t2 = sb("t2", [P, 768], F32)
            rot = sb("rot", [P, 768], F32)
            rqk = sb("rqk", [P, 768], BF16)
            rqkT = sb("rqkT", [64, 12, P], BF16)
            rvb = sb("rvb", [P, 384], BF16)
            Sst = sb("Sst", [64, NH, HD], F32)
            Stm = sb("Stm", [64, NH, HD], F32)
            gS = sb("gS", [64, NH, HD], BF16)
            inT = sb("inT", [P, 6, P], BF16)
            osb = sb("osb", [P, 384], F32)
            osq = sb("osq", [P, 384], F32)
            st1 = sb("st1", [P, 6], F32)
            st2 = sb("st2", [P, 6], F32)
            st3 = sb("st3", [P, 6], F32)
            G = sb("G", [P, D], F32)
            mix = sb("mix", [P, D], BF16)
            mss = sb("mss", [P, 12], F32)
            qaug = sb("qaug", [P, 6, 72], BF16)
            kb = sb("kb", [P, 384], BF16)
            qTa = sb("qTa", [72, 6, P], BF16)
            kTa = sb("kTa", [72, 6, SEQ], BF16)
            vaug = sb("vaug", [P, NT, 6, 65], BF16)
            kmf = sb("kmf", [64, 6, 8], F32)
            kmT = sb("kmT", [64, 6, 8], BF16)
            gsb = sb("gsb", [P, 6, 8], F32)
            m8 = sb("m8", [P, 6, 8], F32)
            sel = sb("sel", [P, 6, 8], F32)
            PT = [sb("PT%d" % i, [P, 4, P], BF16) for i in range(2)]
            rden = sb("rden", [P, 6], F32)
            ub = [sb("ub%d" % i, [P, 256], BF16) for i in range(2)]
            pT = sb("pT", [65, 4, P], BF16)
            pos = sb("pos", [P, 256], F32)
            tg = "p%d_" % l

            S.op("pool", lambda e: e.memset(Sst[:].rearrange("p h e -> p (h e)"), 0.0), writes=[tg + "S"])
            S.op("pool", lambda e: e.memset(gS[:].rearrange("p h e -> p (h e)"), 0.0), writes=[tg + "gS"])
            S.op("pool", lambda e: e.memset(vaug[:, :, :, 64:65], 1.0), writes=[tg + "vaug1"])
            S.op("pool", lambda e: e.memset(pT[64:65, :, :], 1.0), writes=[tg + "pT1"])
            for h in range(6):
                S.dma("sp", lambda e, h=h: e.dma_start(out=kTa[64:72, h, :], in_=blkd),
                      writes=[tg + "kTaI"], key="dkTaI")
            S.op("pool", lambda e: e.memset(qaug[:, :, 64:72], 0.0), writes=[tg + "qaugm"])

            def load_x(t):
                src = xp if l == 0 else y1s
                S.dma("sp", lambda e: e.dma_start(out=xt[t % 2][:], in_=src[t * 128:(t + 1) * 128, :]),
                      reads=(["y1s"] if l > 0 else []), writes=[tg + "xt%d" % (t % 2)])
                S.dma("sp", lambda e: e.dma_start(out=cst[t % 2][:], in_=roped[t * 128:(t + 1) * 128, :]), writes=[tg + "cs%d" % (t % 2)])

            load_x(0)
            for t in range(NT):
                xb = xt[t % 2]
                xk = tg + "xt%d" % (t % 2)
                csb = cst[t % 2]
                ck_ = tg + "cs%d" % (t % 2)
                S.op("act", lambda e, xb=xb: e.activation(out=hb[:], in_=xb[:], func=AF.Square, accum_out=ssq[:, 0:1]),
                     reads=[xk], writes=[tg + "ssq", tg + "hb"])
                rsq(ssq[:, 2:3], ssq[:, 0:1], 1.0 / D, EPS, [tg + "ssq"], [tg + "rstd"])
                rstd = ssq[:, 2:3]
                nrstd = ssq[:, 3:4]
                ts("dve", nrstd, rstd, -1.0, None, ALU.mult, None, [tg + "rstd"], [tg + "rstd"])
                tt("dve", hb[:], xb[:], nwb[:], ALU.mult, [xk, "nwb"], [tg + "hb"])
                for kc in range(8):
                    S.op("pe", lambda e, kc=kc: e.transpose(out=pbf(2)[:, kc * 128:(kc + 1) * 128], in_=hb[:, kc * 128:(kc + 1) * 128], identity=C16("ident")),
                         reads=[tg + "hb", "c16"], writes=["pb2"], mark=(kc == 7))
                S.op("act", lambda e: e.copy(out=hT[:].rearrange("p k t -> p (k t)"), in_=pbf(2)[:, 0:1024]), reads=["pb2"], writes=[tg + "hT"])
                for n in range(7):
                    bk = n % 2
                    for kc in range(8):
                        S.op("pe", lambda e, kc=kc, n=n, bk=bk: e.matmul(PB[bk][:, :], lhsT=hT[:, kc, :], rhs=win[:, kc, n * 512:(n + 1) * 512], start=(kc == 0), stop=(kc == 7)),
                             reads=[tg + "hT", "win"], writes=["pb%d" % bk], mark=(kc == 7), c=0.216)
                    if n < 5:
                        S.op("act", lambda e, n=n, bk=bk: e.activation(out=zs[:, n * 512:(n + 1) * 512], in_=PB[bk][:, :], func=AF.Copy, scale=rstd),
                             reads=["pb%d" % bk, tg + "rstd"], writes=[tg + "zs"], c=0.8)
                    else:
                        gs = G[:, (n - 5) * 512:(n - 4) * 512]
                        gk = tg + "G%d" % (n - 5)
                        S.op("act", lambda e, gs=gs, bk=bk: e.activation(out=gs, in_=PB[bk][:, :], func=AF.Exp, scale=nrstd),
                             reads=["pb%d" % bk, tg + "rstd"], writes=[gk])
                        S.op("act", lambda e, gs=gs: e.activation(out=gs, in_=gs, func=AF.Ln, bias=1.0), reads=[gk], writes=[gk])
                        S.op("act", lambda e, gs=gs: e.activation(out=gs, in_=gs, func=AF.Exp, scale=-1.0), reads=[gk], writes=[gk])
                        S.op("dve", lambda e, gs=gs, bk=bk: e.scalar_tensor_tensor(out=gs, in0=PB[bk][:, :], scalar=rstd, in1=gs, op0=ALU.mult, op1=ALU.mult),
                             reads=["pb%d" % bk, tg + "rstd", gk], writes=[gk])
                    pump(1)
                if t + 1 < NT:
                    load_x(t + 1)
                S.dma("sp", lambda e, t=t: e.dma_start(out=vp[l, t * 128:(t + 1) * 128, :], in_=zs[:, O_MV:O_MV + 384]), reads=[tg + "zs"], key="o_vp")
                if t == NT - 1:
                    S.dma("sp", lambda e: e.dma_start(out=pp[l], in_=zs[113:128, O_PU:O_PU + 256]), reads=[tg + "zs"], key="o_pp")
                pump(2)
                rope(P, zs[:, 0:768], csb, t1[:], t2[:], rot[:], tg + "r", [tg + "zs", ck_], okey=tg + "rot")
                tt("dve", rqk[:].rearrange("p (h d) -> p h d", h=12), rot[:].rearrange("p (h d) -> p h d", h=12),
                   C32("gqk").unsqueeze(2).to_broadcast([P, 12, 64]), ALU.mult, [tg + "rot", "c32"], [tg + "rqk"])
                S.op("act", lambda e: e.copy(out=rvb[:], in_=zs[:, O_RV:O_RV + 384]), reads=[tg + "zs"], writes=[tg + "rvb"])
                for half in range(2):
                    for hh in range(6):
                        h = half * 6 + hh
                        S.op("pe", lambda e, h=h, hh=hh: e.transpose(out=pbf(2)[0:64, hh * 128:(hh + 1) * 128], in_=rqk[:, h * 64:(h + 1) * 64], identity=C16("ident")),
                             reads=[tg + "rqk", "c16"], writes=["pb2"], mark=(hh == 5))
                    S.op("act", lambda e, half=half: e.copy(out=rqkT[:, half * 6:(half + 1) * 6, :].rearrange("p h t -> p (h t)"), in_=pbf(2)[0:64, 0:768]),
                         reads=["pb2"], writes=[tg + "rqkT"])
                pump(2)
                for hg in range(2):
                    for hh in range(3):
                        h = hg * 3 + hh
                        S.op("pe", lambda e, h=h, hh=hh: e.matmul(PB[4][:, hh * 128:(hh + 1) * 128], lhsT=rqkT[:, 6 + h, :], rhs=rqkT[:, h, :], start=True, stop=True),
                             reads=[tg + "rqkT"], writes=["pb4"], mark=(hh == 2))
                    tt("dve", inT[:, hg * 3:(hg + 1) * 3, :], PB[4][:, 0:384].rearrange("p (h t) -> p h t", h=3),
                       C32("mask01T").unsqueeze(1).to_broadcast([P, 3, P]), ALU.mult, ["pb4", "c32"], [tg + "inT"])
                for h in range(6):
                    S.op("pe", lambda e, h=h: e.matmul(PB[5][:, h * 64:(h + 1) * 64], lhsT=inT[:, h, :], rhs=rvb[:, h * 64:(h + 1) * 64], start=True, stop=False),
                         reads=[tg + "inT", tg + "rvb"], writes=["pb5"], mark=False)
                    S.op("pe", lambda e, h=h: e.matmul(PB[5][:, h * 64:(h + 1) * 64], lhsT=rqkT[:, h, :], rhs=gS[:, h, :], start=False, stop=True),
                         reads=[tg + "rqkT", tg + "gS"], writes=["pb5"], mark=(h == 5))
                pump(2)
                for h in range(6):
                    S.op("pe", lambda e, h=h: e.matmul(PB[4][0:64, h * 64:(h + 1) * 64], lhsT=rqk[:, 384 + h * 64:384 + (h + 1) * 64], rhs=rvb[:, h * 64:(h + 1) * 64], start=True, stop=True),
                         reads=[tg + "rqk", tg + "rvb"], writes=["pb4"], mark=(h == 5))
                tt("dve", Stm[:], PB[4][0:64, 0:384].rearrange("p (h e) -> p h e", h=6),
                   C32("dec", 64)[:, 6:12].unsqueeze(2).to_broadcast([64, NH, HD]), ALU.mult, ["pb4", "c32"], [tg + "Stm"])
                tt("dve", Sst[:], Sst[:], C32("dec", 64)[:, 0:6].unsqueeze(2).to_broadcast([64, NH, HD]), ALU.mult, [tg + "S", "c32"], [tg + "S"])
                tt("dve", Sst[:], Sst[:], Stm[:], ALU.add, [tg + "S", tg + "Stm"], [tg + "S"])
                tt("dve", gS[:], Sst[:], C32("dec", 64)[:, 12:18].unsqueeze(2).to_broadcast([64, NH, HD]), ALU.mult, [tg + "S", "c32"], [tg + "gS"])
                if t == NT - 1:
                    S.dma("sp", lambda e: e.dma_start(out=rp[l].rearrange("h d e -> d h e"), in_=Sst[:]), reads=[tg + "S"], key="o_rp")
                group_norm(P, PB[5][:, 0:384], "pb5", osb, osq, st1, st2, st3, G[:, 0:384], mix[:, 0:384], tg, gkeys=[tg + "G0", tg + "G1"])
                pump(2)
                ubc = ub[t % 2]
                ubp = ub[(t + 1) % 2]
                S.op("act", lambda e, ubc=ubc: e.copy(out=ubc[:], in_=zs[:, O_PU:O_PU + 256]), reads=[tg + "zs"], writes=[tg + "ub%d" % (t % 2)])
                Acur = C16("A0") if t == 0 else C16("A1")
                for g in range(4):
                    S.op("pe", lambda e, g=g, ubc=ubc, Acur=Acur: e.matmul(PB[4][0:64, g * 128:(g + 1) * 128], lhsT=ubc[:, g * 64:(g + 1) * 64], rhs=Acur[:, g * 128:(g + 1) * 128], start=True, stop=(t == 0)),
                         reads=[tg + "ub%d" % (t % 2), "c16"], writes=["pb4"], mark=(t == 0 and g == 3))
                    if t > 0:
                        S.op("pe", lambda e, g=g, ubp=ubp: e.matmul(PB[4][0:64, g * 128:(g + 1) * 128], lhsT=ubp[:, g * 64:(g + 1) * 64], rhs=C16("AP")[:, g * 128:(g + 1) * 128], start=False, stop=True),
                             reads=[tg + "ub%d" % ((t + 1) % 2), "c16"], writes=["pb4"], mark=(g == 3))
                S.op("act", lambda e: e.copy(out=pT[0:64, :, :].rearrange("p g t -> p (g t)"), in_=PB[4][0:64, 0:512]), reads=["pb4"], writes=[tg + "pT"])
                for g in range(4):
                    S.op("pe", lambda e, g=g: e.matmul(PB[5][:, g * 64:(g + 1) * 64], lhsT=pT[0:65, g, :], rhs=pwa[0:65, g, :], start=True, stop=True),
                         reads=[tg + "pT", tg + "pT1", "pwa"], writes=["pb5"], mark=(g == 3))
                tt("dve", pos[:], PB[5][:, 0:256], psb[:], ALU.mult, ["pb5", "psb"], [tg + "pos"])
                tt("dve", mix[:, 768:1024], pos[:], G[:, 768:1024], ALU.mult, [tg + "pos", tg + "G0", tg + "G1"], [tg + "mixp"])
                pump(2)
                qk_norm(P, zs[:, O_MQ:O_MQ + 768], tg + "zs", None, osq_big=t1, mss=mss, tag=tg)
                rope(P, zs[:, O_MQ:O_MQ + 768], csb, t1[:], t2[:], rot[:], tg + "m", [tg + "zs", ck_], okey=tg + "rot")
                S.dma("sp", lambda e, t=t: e.dma_start(out=kp[l, t * 128:(t + 1) * 128, :], in_=rot[:, 384:768]), reads=[tg + "rot"], key="o_kp")
                S.op("dve", lambda e: e.tensor_copy(out=qaug[:, :, 0:64], in_=rot[:, 0:384].rearrange("p (h d) -> p h d", h=6)), reads=[tg + "rot"], writes=[tg + "qaug"])
                S.op("act", lambda e: e.copy(out=kb[:], in_=rot[:, 384:768]), reads=[tg + "rot"], writes=[tg + "kb"])
                S.op("act", lambda e, t=t: e.copy(out=vaug[:, t, :, 0:64], in_=zs[:, O_MV:O_MV + 384].rearrange("p (h d) -> p h d", h=6)), reads=[tg + "zs"], writes=[tg + "vaug%d" % t])
                pump(2)
                for h in range(6):
                    S.op("pe", lambda e, h=h: e.transpose(out=pbf(2)[0:64, h * 128:(h + 1) * 128], in_=kb[:, h * 64:(h + 1) * 64], identity=C16("ident")),
                         reads=[tg + "kb", "c16"], writes=["pb2"], mark=(h == 5))
                S.op("act", lambda e, t=t: e.copy(out=kTa[0:64, :, t * 128:(t + 1) * 128], in_=pbf(2)[0:64, 0:768].rearrange("p (h t) -> p h t", h=6)),
                     reads=["pb2"], writes=[tg + "kTa%d" % t])
                blk = t // 2
                if blk >= 4:
                    for h in range(6):
                        S.op("pe", lambda e, h=h: e.transpose(out=pbf(2)[0:64, h * 128:(h + 1) * 128], in_=qaug[:, h, 0:64], identity=C16("ident")),
                             reads=[tg + "qaug", "c16"], writes=["pb2"], mark=(h == 5))
                    S.op("dve", lambda e: e.tensor_copy(out=qTa[0:64, :, :].rearrange("p h t -> p (h t)"), in_=pbf(2)[0:64, 0:768]), reads=["pb2"], writes=[tg + "qTa"])
                    for h in range(6):
                        S.op("pe", lambda e, h=h: e.matmul(PB[4][:, h * 8:(h + 1) * 8], lhsT=qTa[0:64, h, :], rhs=kmT[:, h, :], start=True, stop=True),
                             reads=[tg + "qTa", tg + "kmT"], writes=["pb4"], mark=(h == 5))
                    if True:
                        S.op("dve", lambda e: e.memset(gsb[:].rearrange("p h c -> p (h c)"), -1e30), writes=[tg + "gsb"])
                        S.op("dve", lambda e, blk=blk: e.tensor_copy(out=gsb[:, :, 0:blk], in_=PB[4][:, 0:48].rearrange("p (h c) -> p h c", h=6)[:, :, 0:blk]),
                             reads=["pb4"], writes=[tg + "gsb"])
                        for h in range(6):
                            S.op("dve", lambda e, h=h: e.max(out=m8[:, h, :], in_=gsb[:, h, :]), reads=[tg + "gsb"], writes=[tg + "m8"])
                        tt("dve", sel[:], gsb[:], m8[:, :, 2:3].to_broadcast([P, 6, 8]), ALU.is_ge, [tg + "gsb", tg + "m8"], [tg + "sel"])
                        ts("dve", sel[:], sel[:], -1.0, -NEG, ALU.add, ALU.mult, [tg + "sel"], [tg + "sel"])
                        S.op("dve", lambda e, blk=blk: e.tensor_copy(out=qaug[:, :, 64:64 + blk], in_=sel[:, :, 0:blk]), reads=[tg + "sel"], writes=[tg + "qaugm"])
                pump(2)
                for h in range(6):
                    S.op("pe", lambda e, h=h: e.transpose(out=pbf(2)[0:72, h * 128:(h + 1) * 128], in_=qaug[:, h, :], identity=C16("ident")),
                         reads=[tg + "qaug", tg + "qaugm", "c16"], writes=["pb2"], mark=(h == 5))
                S.op("act", lambda e: e.copy(out=qTa[:].rearrange("p h t -> p (h t)"), in_=pbf(2)[0:72, 0:768]), reads=["pb2"], writes=[tg + "qTa"])
                if t % 2 == 1:
                    S.op("dve", lambda e, blk=blk: e.tensor_reduce(out=kmf[:, :, blk], in_=kTa[0:64, :, blk * 256:(blk + 1) * 256], axis=AX.X, op=ALU.add),
                         reads=[tg + "kTa%d" % (2 * blk), tg + "kTa%d" % (2 * blk + 1)], writes=[tg + "kmf"])
                    S.op("dve", lambda e, blk=blk: e.tensor_copy(out=kmT[:, :, blk], in_=kmf[:, :, blk]), reads=[tg + "kmf"], writes=[tg + "kmT"])
                pump(2)
                groups = []
                for h in range(6):
                    for g0 in range(0, t + 1, 4):
                        groups.append((h, list(range(g0, min(g0 + 4, t + 1)))))

                def emit_qk(i):
                    h, kts = groups[i]
                    bank = (3, 6)[i % 2]
                    for j, kt in enumerate(kts):
                        diag = (kt == t)
                        S.op("pe", lambda e, h=h, j=j, kt=kt, bank=bank, diag=diag: e.matmul(PB[bank][:, j * 128:(j + 1) * 128], lhsT=kTa[0:72, h, kt * 128:(kt + 1) * 128], rhs=qTa[0:72, h, :], start=True, stop=(not diag)),
                             reads=[tg + "kTa%d" % kt, tg + "kTaI", tg + "qTa"], writes=["pb%d" % bank], mark=(j == len(kts) - 1 and not diag))
                        if diag:
                            S.op("pe", lambda e, j=j, bank=bank: e.matmul(PB[bank][:, j * 128:(j + 1) * 128], lhsT=C16("ident"), rhs=C16("trineg"), start=False, stop=True),
                                 reads=["c16"], writes=["pb%d" % bank], mark=True)

                emit_qk(0)
                for i, (h, kts) in enumerate(groups):
                    if i + 1 < len(groups):
                        emit_qk(i + 1)
                    if i % max(1, len(groups) // 6) == 0:
                        pump(2)
                    bank = (3, 6)[i % 2]
                    ptb = PT[i % 2]
                    ptk = tg + "PT%d" % (i % 2)
                    nk = len(kts)
                    S.op("act", lambda e, nk=nk, bank=bank, ptb=ptb: e.activation(out=ptb[:, 0:nk, :].rearrange("p a q -> p (a q)"), in_=PB[bank][:, 0:nk * 128], func=AF.Exp, scale=0.125),
                         reads=["pb%d" % bank], writes=[ptk], c=0.12 + 0.13 * nk)
                    for j, kt in enumerate(kts):
                        S.op("pe", lambda e, h=h, j=j, kt=kt, ptb=ptb: e.matmul(PB[5][:, h * 65:(h + 1) * 65], lhsT=ptb[:, j, :], rhs=vaug[:, kt, h, :], start=(kt == 0), stop=(kt == t)),
                             reads=[ptk, tg + "vaug%d" % kt, tg + "vaug1"], writes=["pb5"], mark=(j == len(kts) - 1))
                pvv = PB[5][:, 0:390].rearrange("p (h e) -> p h e", h=6)
                S.op("dve", lambda e: e.reciprocal(out=rden[:].unsqueeze(2), in_=pvv[:, :, 64:65]), reads=["pb5"], writes=[tg + "rden"])
                tt("dve", osb[:].rearrange("p (h e) -> p h e", h=6), pvv[:, :, 0:64], rden[:].unsqueeze(2).to_broadcast([P, 6, 64]), ALU.mult, ["pb5", tg + "rden"], [tg + "osb"])
                tt("dve", mix[:, 384:768], osb[:], G[:, 384:768], ALU.mult, [tg + "osb", tg + "G0", tg + "G1"], [tg + "mixm"])
                pump(2)
                for kc in range(8):
                    S.op("pe", lambda e, kc=kc: e.transpose(out=pbf(2)[:, kc * 128:(kc + 1) * 128], in_=mix[:, kc * 128:(kc + 1) * 128], identity=C16("ident")),
                         reads=[tg + "mixr", tg + "mixm", tg + "mixp", "c16"], writes=["pb2"], mark=(kc == 7))
                S.op("dve", lambda e: e.tensor_copy(out=hT[:].rearrange("p k t -> p (k t)"), in_=pbf(2)[:, 0:1024]), reads=["pb2"], writes=[tg + "hT"])
                ytb = xb
                yk = xk
                for n in range(2):
                    for kc in range(8):
                        S.op("pe", lambda e, kc=kc, n=n: e.matmul(PB[n][:, :], lhsT=hT[:, kc, :], rhs=wout[:, kc, n * 512:(n + 1) * 512], start=(kc == 0), stop=(kc == 7)),
                             reads=[tg + "hT", "wout"], writes=["pb%d" % n], mark=(kc == 7), c=0.216)
                    tt("dve", ytb[:, n * 512:(n + 1) * 512], PB[n][:, :], xb[:, n * 512:(n + 1) * 512], ALU.add, ["pb%d" % n, xk], [yk])
                dst = y1s if l == 0 else yp
                S.dma("sp", lambda e, t=t, ytb=ytb, dst=dst: e.dma_start(out=dst[t * 128:(t + 1) * 128, :], in_=ytb[:]),
                      reads=[yk], writes=(["y1s"] if l == 0 else []), key=("dy1" if l == 0 else "o_yp"))

        for l in range(DEPTH):
            load_weights(l)
            with ExitStack() as kst:
                gen = sample_layer(l, kst)
                assert next(gen) == "pre"
                state = {"done": False}

                def pump(n):
                    for _ in range(n):
                        if state["done"]:
                            return
                        if next(gen) == "pages":
                            state["done"] = True

                with ExitStack() as ps_:
                    prompt_layer(l, ps_, pump)
                    while not state["done"]:
                        pump(1)
                    S.barrier()
                assert next(gen) == "post"
        S.emit()
    return nc


_CACHE = {}


def kernel(x_prompt, x_sample, cache_k, cache_v, state_ret, state_pool, page_table,
           norm_w, w_in, w_out, ret_gn_w, q_norm_w, k_norm_w, pool_w, pool_b, pool_scale):
    f = lambda a: np.ascontiguousarray(np.asarray(a, dtype=np.float32))
    x_prompt, x_sample, cache_k, cache_v = f(x_prompt), f(x_sample), f(cache_k), f(cache_v)
    state_ret, state_pool = f(state_ret), f(state_pool)
    page_table = np.ascontiguousarray(np.asarray(page_table, dtype=np.int32))
    if "c" not in _CACHE:
        _CACHE["c"] = _consts()
    o16, a16, o32, a32, (rope_p, blkind) = _CACHE["c"]
    nc = build_nc(o16, a16.shape[1], o32, a32.shape[1])
    ckr = cache_k.reshape(DEPTH, NPOOL, 128, 384)
    cvr = cache_v.reshape(DEPTH, NPOOL, 128, 384)
    common = dict(ck=ckr, cv=cvr, norm_w=f(norm_w), w_in=f(w_in), w_out=f(w_out), gn_w=f(ret_gn_w),
                  qn_w=f(q_norm_w), kn_w=f(k_norm_w), pool_w=f(pool_w), pool_b=f(pool_b), pool_s=f(pool_scale),
                  c16=a16, c32=a32, rope_p=rope_p, blkind=blkind)
    in_maps = []
    for c in range(NCORES):
        m = dict(common)
        m["xp"] = x_prompt[c]
        m["xs"] = x_sample[4 * c:4 * c + 4].reshape(16, D)
        m["sret"] = np.ascontiguousarray(state_ret[:, 4 * c:4 * c + 4])
        m["spool"] = np.ascontiguousarray(state_pool[:, 4 * c:4 * c + 4].reshape(DEPTH, 60, 256))
        m["ptab"] = np.ascontiguousarray(page_table[4 * c:4 * c + 4].reshape(1, 256))
        in_maps.append(m)
    res = run_bass_kernel_spmd(nc, in_maps, core_ids=list(range(NCORES)))
    R = res.results
    y_p = np.stack([R[c]["yp"] for c in range(NCORES)]).reshape(8, SEQ, D)
    y_s = np.concatenate([R[c]["ys"].reshape(4, 4, D) for c in range(NCORES)], axis=0)
    k_p = np.stack([R[c]["kp"] for c in range(NCORES)], axis=1).reshape(DEPTH, 8, SEQ, NH, HD)
    v_p = np.stack([R[c]["vp"] for c in range(NCORES)], axis=1).reshape(DEPTH, 8, SEQ, NH, HD)
    k_s = np.concatenate([R[c]["ks"].reshape(DEPTH, 4, 4, NH, HD) for c in range(NCORES)], axis=1)
    v_s = np.concatenate([R[c]["vs"].reshape(DEPTH, 4, 4, NH, HD) for c in range(NCORES)], axis=1)
    r_p = np.stack([R[c]["rp"] for c in range(NCORES)], axis=1)
    r_s = np.concatenate([R[c]["rs"] for c in range(NCORES)], axis=1)
    p_p = np.stack([R[c]["pp"] for c in range(NCORES)], axis=1)
    p_s = np.concatenate([R[c]["pso"] for c in range(NCORES)], axis=1)
    outs = (y_p, y_s, k_p, v_p, k_s, v_s, r_p, r_s, p_p, p_s)
    return tuple(np.ascontiguousarray(o, dtype=np.float32) for o in outs)
```

```python
import numpy as np
import ml_dtypes
from contextlib import ExitStack
import concourse.bass as bass
import concourse.mybir as mybir
from concourse.bass_utils import run_bass_kernel_spmd

F32 = mybir.dt.float32
BF16 = mybir.dt.bfloat16
I32 = mybir.dt.int32
ALU = mybir.AluOpType
AF = mybir.ActivationFunctionType
AX = mybir.AxisListType

NCORES = 8
D = 1024
SEQ = 2048
NT = SEQ // 128
DEPTH = 2
DIN = 3584
HD = 64
NH = 6
NPOOL = 2560
NPAGES = 64
EPS = 1e-6
NEG = -30000.0
SAME_SYNC = True

O_RQ, O_RK, O_RV, O_MQ, O_MK, O_MV, O_PU, O_RG, O_MG, O_PG = 0, 384, 768, 1152, 1536, 1920, 2304, 2560, 2944, 3328
WIN_SEGS = ((0, 1152, 0), (1152, 384, 2560), (1536, 1152, 1152), (2688, 384, 2944), (3072, 256, 2304), (3328, 256, 3328))


DEF_COST = {"pe": 0.09, "act": 0.55, "dve": 0.6, "pool": 1.0, "sp": 0.1}
SYNC_LAT = 0.08
PRIORITY_MODE = "rank"


class Sched:
    def __init__(self, nc, stack):
        self.nc = nc
        self.stack = stack
        self.names = ["pe", "act", "dve", "pool", "sp"]
        self.sem = {n: stack.enter_context(nc.semaphore("s_" + n)) for n in self.names}
        self.dsem = {}
        self.nodes = []
        self.res_w = {}
        self.res_r = {}
        self.pending = None
        self.epoch = 0
        self.last_dma_of_key = {}
        self.pool_dmas = []

    def _new_node(self, eng, kind):
        nd = dict(id=len(self.nodes), eng=eng, kind=kind, fns=[], cost=0.0, reads=set(), writes=set(),
                  preds=set(), epoch=self.epoch, key=None, val=0, lat=0.0)
        return nd

    def _finalize(self, nd):
        reads = set(nd["reads"])
        writes = set(nd["writes"])
        for r in list(reads):
            if r.startswith("pb"):
                writes.add(r)
        preds = nd["preds"]
        for r in reads:
            w = self.res_w.get(r)
            if w is not None:
                preds.add(w)
        for w_ in writes:
            w = self.res_w.get(w_)
            if w is not None:
                preds.add(w)
            for t in self.res_r.get(w_, ()):
                preds.add(t)
        nd["id"] = len(self.nodes)
        preds.discard(nd["id"])
        self.nodes.append(nd)
        for r in reads:
            self.res_r.setdefault(r, []).append(nd["id"])
        for w_ in writes:
            self.res_w[w_] = nd["id"]
            self.res_r[w_] = []

    def op(self, eng, fn, reads=(), writes=(), mark=True, c=None):
        if self.pending is not None and self.pending["eng"] != eng:
            raise RuntimeError("unmarked group interrupted")
        nd = self.pending if self.pending is not None else self._new_node(eng, "op")
        nd["fns"].append((fn, mark))
        nd["cost"] += (c if c is not None else DEF_COST[eng])
        nd["reads"].update(reads)
        nd["writes"].update(writes)
        if mark:
            self.pending = None
            self._finalize(nd)
        else:
            self.pending = nd

    def dma(self, q, fn, reads=(), writes=(), key=None, c=None, lat=None):
        assert self.pending is None
        k = key if key is not None else (writes[0] if writes else reads[0])
        if k not in self.dsem:
            s = self.stack.enter_context(self.nc.semaphore("d%d" % len(self.dsem)))
            self.dsem[k] = [s, 0]
        ent = self.dsem[k]
        ent[1] += 16
        nd = self._new_node(q, "dma")
        nd["fns"].append((fn, False))
        nd["cost"] = c if c is not None else (1.2 if q == "pool" else 0.1)
        nd["lat"] = lat if lat is not None else 3.0
        nd["reads"].update(reads)
        nd["writes"].update(writes)
        nd["key"] = k
        nd["val"] = ent[1]
        prev = self.last_dma_of_key.get(k)
        if prev is not None:
            nd["preds"].add(("issue", prev))
        if q == "pool":
            if len(self.pool_dmas) >= 10:
                nd["preds"].add(self.pool_dmas[-10])
        self._finalize(nd)
        self.last_dma_of_key[k] = nd["id"]
        if q == "pool":
            self.pool_dmas.append(nd["id"])

    def barrier(self):
        assert self.pending is None
        self.epoch += 1

    def _schedule(self):
        import heapq
        nodes = self.nodes
        n = len(nodes)
        succs = [[] for _ in range(n)]
        npred = [0] * n
        for nd in nodes:
            ps = set()
            for p in nd["preds"]:
                if isinstance(p, tuple):
                    ps.add((p[1], True))
                else:
                    ps.add((p, False))
            norm = {p for p, iss in ps if not iss}
            plist = [(p, False) for p in norm] + [(p, True) for p, iss in ps if iss and p not in norm]
            nd["plist"] = plist
            npred[nd["id"]] = len(plist)
            for p, iss in plist:
                succs[p].append((nd["id"], iss))
        rank = [0.0] * n
        for nd in reversed(nodes):
            i = nd["id"]
            best = 0.0
            for sidx, iss in succs[i]:
                if nodes[sidx]["epoch"] == nd["epoch"] and rank[sidx] > best:
                    best = rank[sidx]
            rank[i] = best + nd["cost"] + (nd["lat"] if nd["kind"] == "dma" else 0.0)
        PRI = PRIORITY_MODE
        order = {e: [] for e in self.names}
        ready_t = [0.0] * n
        eng_free = {e: 0.0 for e in self.names}
        fut = {e: [] for e in self.names}
        rdy = {e: [] for e in self.names}
        epoch_nodes = {}
        for nd in nodes:
            epoch_nodes.setdefault(nd["epoch"], []).append(nd["id"])
        t_base = 0.0
        dma_free = 0.0
        fin = [0.0] * n
        issue_fin = [0.0] * n
        for ep in sorted(epoch_nodes):
            ids = epoch_nodes[ep]
            idset = set(ids)
            left = len(ids)
            for e in self.names:
                eng_free[e] = max(eng_free[e], t_base)
            for i in ids:
                cnt = 0
                for p, iss in nodes[i]["plist"]:
                    if p in idset:
                        cnt += 1
                npred[i] = cnt
                ready_t[i] = t_base
                if cnt == 0:
                    heapq.heappush(fut[nodes[i]["eng"]], (t_base, i))
            while left > 0:
                best = None
                for e in self.names:
                    f = fut[e]
                    r = rdy[e]
                    while f and f[0][0] <= eng_free[e]:
                        j_ = heapq.heappop(f)[1]
                        heapq.heappush(r, ((-rank[j_], j_) if PRI == "rank" else (0.0, j_)))
                    if r:
                        cand = (eng_free[e], r[0][1], e, True)
                    elif f:
                        cand = (f[0][0], f[0][1], e, False)
                    else:
                        continue
                    if best is None or cand[:2] < best[:2]:
                        best = cand
                assert best is not None, "scheduler stuck (dependency cycle?)"
                start, i, e, from_r = best
                if from_r:
                    heapq.heappop(rdy[e])
                else:
                    heapq.heappop(fut[e])
                nd = nodes[i]
                order[e].append(i)
                issue_fin[i] = start + nd["cost"]
                eng_free[e] = issue_fin[i]
                if nd["kind"] == "dma":
                    xfer = max(issue_fin[i], dma_free) + 0.4
                    dma_free = xfer
                    fin[i] = xfer + nd["lat"]
                else:
                    fin[i] = issue_fin[i]
                left -= 1
                for sidx, iss in succs[i]:
                    if sidx not in idset:
                        continue
                    sn = nodes[sidx]
                    t = issue_fin[i] if iss else fin[i]
                    if sn["eng"] != e or nd["kind"] == "dma":
                        t += SYNC_LAT
                    if t > ready_t[sidx]:
                        ready_t[sidx] = t
                    npred[sidx] -= 1
                    if npred[sidx] == 0:
                        heapq.heappush(fut[sn["eng"]], (ready_t[sidx], sidx))
            t_base = max([t_base] + [fin[i] for i in ids])
        self.makespan = t_base
        return order

    def emit(self):
        nc = self.nc
        assert self.pending is None
        nodes = self.nodes
        order = self._schedule()
        cnt = {}
        for e in self.names:
            c = 0
            for i in order[e]:
                if nodes[i]["kind"] == "op":
                    c += 1
                    cnt[i] = c
        ep_eng = {}
        ep_dma = {}
        for nd in nodes:
            ep = nd["epoch"]
            if nd["kind"] == "op":
                d = ep_eng.setdefault(ep, {})
                d[nd["eng"]] = max(d.get(nd["eng"], 0), cnt[nd["id"]])
            else:
                d = ep_dma.setdefault(ep, {})
                d[nd["key"]] = max(d.get(nd["key"], 0), nd["val"])
        prog = {e: [] for e in self.names}
        for e in self.names:
            waited = {}
            cur_ep = 0

            def need(kind, src, val):
                if kind == "eng" and src == e and (e in ("pe", "sp") or not SAME_SYNC):
                    return
                if val <= 0:
                    return
                k = (kind, src)
                if waited.get(k, 0) >= val:
                    return
                waited[k] = val
                prog[e].append(("wait", kind, src, val))

            for i in order[e]:
                nd = nodes[i]
                if nd["epoch"] != cur_ep:
                    for ep in range(cur_ep, nd["epoch"]):
                        for src, v in ep_eng.get(ep, {}).items():
                            if src != e:
                                need("eng", src, v)
                        for k, v in ep_dma.get(ep, {}).items():
                            need("dma", k, v)
                    cur_ep = nd["epoch"]
                for p, iss in nd["plist"]:
                    if iss:
                        continue
                    pn = nodes[p]
                    if pn["kind"] == "op":
                        need("eng", pn["eng"], cnt[p])
                    else:
                        need("dma", pn["key"], pn["val"])
                if nd["kind"] == "op":
                    for fn, mark in nd["fns"]:
                        prog[e].append(("op", fn, mark))
                else:
                    prog[e].append(("dma", nd["fns"][0][0], self.dsem[nd["key"]][0]))
            if e == "sp":
                for k, v in self.dsem.items():
                    need("dma", k, v[1])
                for src in self.names:
                    tot = max([0] + [cnt[i] for i in order[src] if nodes[i]["kind"] == "op"])
                    if src != "sp":
                        need("eng", src, tot)

        def replay(name, eng):
            for item in prog[name]:
                if item[0] == "wait":
                    _, kind, src, val = item
                    s = self.sem[src] if kind == "eng" else self.dsem[src][0]
                    eng.wait_ge(s, val)
                elif item[0] == "op":
                    ins = item[1](eng)
                    if item[2]:
                        ins.then_inc(self.sem[name], 1)
                else:
                    item[1](eng).then_inc(item[2], 16)

        with nc.Block() as block:

            @block.tensor
            def _(e):
                replay("pe", e)

            @block.scalar
            def _(e):
                replay("act", e)

            @block.vector
            def _(e):
                replay("dve", e)

            @block.gpsimd
            def _(e):
                replay("pool", e)

            @block.sync
            def _(e):
                replay("sp", e)


def _consts():
    g = 1.0 - 2.0 ** (-5.0 - np.arange(NH, dtype=np.float64))
    p = np.arange(128, dtype=np.float64)
    c16 = {}
    c32 = {}
    c16["ident"] = np.eye(128)
    kk = np.arange(128)[:, None]
    qq = np.arange(128)[None, :]
    c16["trineg"] = np.where(kk <= qq, 0.0, NEG)
    wins = (2, 4, 8, 16)
    A0 = np.zeros((128, 4, 128))
    A1 = np.zeros((128, 4, 128))
    AP = np.zeros((128, 4, 128))
    for gi, w in enumerate(wins):
        for t in range(128):
            for s in range(t - w + 1, t + 1):
                if s >= 0:
                    A0[s, gi, t] += 1.0 / min(t + 1, w)
                    A1[s, gi, t] += 1.0 / w
                else:
                    AP[128 + s, gi, t] += 1.0 / w
            A0[t, gi, t] -= 1.0
            A1[t, gi, t] -= 1.0
    c16["A0"] = A0.reshape(128, 512)
    c16["A1"] = A1.reshape(128, 512)
    c16["AP"] = AP.reshape(128, 512)
    Ab = np.zeros((128, 4, 16))
    Ac = np.zeros((128, 4, 16))
    for gi, w in enumerate(wins):
        for b in range(4):
            for q in range(4):
                lo = 15 + q - w + 1
                for idx in range(lo, 15 + q + 1):
                    if idx < 15:
                        Ab[15 * b + idx, gi, 4 * b + q] += 1.0 / w
                    else:
                        Ac[4 * b + idx - 15, gi, 4 * b + q] += 1.0 / w
                Ac[4 * b + q, gi, 4 * b + q] -= 1.0
    c16["Ab"] = Ab.reshape(128, 64)
    c16["Ac"] = Ac.reshape(128, 64)
    blk = np.zeros((128, 2048))
    for c in range(8):
        blk[c, c * 256:(c + 1) * 256] = 1.0
    blkind = blk[:8].astype(ml_dtypes.bfloat16)
    c16["ones"] = np.ones((128, 128))
    dm = np.zeros((128, 390))
    for h in range(6):
        dm[16 * h:16 * h + 16, 65 * h:65 * h + 65] = 1.0
    c16["dmask"] = dm
    c32["mask01T"] = (kk <= qq).astype(np.float64)
    gq = g[None, :] ** p[:, None]
    gk = g[None, :] ** (-p[:, None]) * (HD ** -0.5)
    c32["gqk"] = np.concatenate([gq, gk], axis=1)
    dec = np.zeros((128, 30))
    dec[:, 0:6] = g ** 128
    dec[:, 6:12] = g ** 127
    dec[:, 12:18] = g
    dec[:, 18:24] = g ** 4
    dec[:, 24:30] = g ** 3
    c32["dec"] = dec
    ps = (np.arange(128) % 4).astype(np.float64)
    gqs = g[None, :] ** ps[:, None]
    gks = g[None, :] ** (-ps[:, None]) * (HD ** -0.5)
    c32["gqk_s"] = np.concatenate([gqs, gks], axis=1)
    j = np.arange(16)[:, None]
    i = np.arange(16)[None, :]
    same = (j // 4) == (i // 4)
    m01s = np.zeros((128, 16))
    m01s[:16] = (same & (i >= j)).astype(np.float64)
    c32["mask01s"] = m01s
    om = np.full((128, 16), NEG)
    om[:16] = np.where(same & (j <= i), 0.0, NEG)
    c32["ownmask"] = om
    bm = np.zeros((128, 4, 16))
    for b in range(4):
        bm[:, b, 4 * b:4 * b + 4] = 1.0
    c32["bmask"] = bm.reshape(128, 64)
    rm = np.zeros((128, 4))
    for b in range(4):
        rm[4 * b:4 * b + 4, b] = 1.0
    c32["rowmask"] = rm
    id24 = np.zeros((128, 24))
    id24[:24] = np.eye(24)
    c32["ident24"] = id24
    c32["ones"] = np.ones((128, 8))
    sel = np.zeros((128, 16))
    for r in range(96):
        sel[r, r % 16] = 1.0
    c32["sel96"] = sel
    c32["iota2"] = np.arange(128, dtype=np.float64)[:, None] + np.array([0.0, NPOOL * 128.0])[None, :]
    half = HD // 2
    inv = 1.0 / (10000.0 ** (np.arange(half, dtype=np.float32) / half))

    def tab(pos):
        ang = (pos.astype(np.float32)[:, None] * inv[None, :]).astype(np.float32)
        cos = np.cos(ang).astype(np.float32)
        sin = np.sin(ang).astype(np.float32)
        return np.concatenate([cos, cos, -sin, sin], axis=1).astype(np.float32)

    rope_p = tab(np.arange(SEQ))
    rs = np.zeros((128, 128), np.float32)
    rs[:16] = tab(8192 + (np.arange(16) % 4))
    c32["rope_s"] = rs

    def pack(dct, dt):
        offs = {}
        cols = []
        o = 0
        for k, v in dct.items():
            offs[k] = (o, v.shape[1])
            cols.append(v)
            o += v.shape[1]
        return offs, np.ascontiguousarray(np.concatenate(cols, axis=1)).astype(dt)

    o16, a16 = pack(c16, ml_dtypes.bfloat16)
    o32, a32 = pack(c32, np.float32)
    return o16, a16, o32, a32, (rope_p, blkind)


def build_nc(o16, n16, o32, n32):
    nc = bass.Bass("TRN2", target_bir_lowering=False)
    dt_in = lambda name, shape, dt=F32: nc.dram_tensor(name, shape, dt, kind="ExternalInput").ap()
    dt_out = lambda name, shape, dt=F32: nc.dram_tensor(name, shape, dt, kind="ExternalOutput").ap()
    xp = dt_in("xp", [SEQ, D])
    xs_d = dt_in("xs", [16, D])
    ck = dt_in("ck", [DEPTH, NPOOL, 128, 384])
    cv = dt_in("cv", [DEPTH, NPOOL, 128, 384])
    sret = dt_in("sret", [DEPTH, 4, NH, HD, HD])
    spool = dt_in("spool", [DEPTH, 60, 256])
    ptab = dt_in("ptab", [1, 256], I32)
    norm_w = dt_in("norm_w", [DEPTH, D])
    w_in = dt_in("w_in", [DEPTH, D, DIN])
    w_out = dt_in("w_out", [DEPTH, D, D])
    gn_w = dt_in("gn_w", [DEPTH, 384])
    qn_w = dt_in("qn_w", [DEPTH, HD])
    kn_w = dt_in("kn_w", [DEPTH, HD])
    pool_w = dt_in("pool_w", [DEPTH, 4, HD, HD])
    pool_b = dt_in("pool_b", [DEPTH, 4, HD])
    pool_s = dt_in("pool_s", [DEPTH, 256])
    c16d = dt_in("c16", [128, n16], BF16)
    c32d = dt_in("c32", [128, n32])
    roped = dt_in("rope_p", [SEQ, 128])
    blkd = dt_in("blkind", [8, SEQ], BF16)

    yp = dt_out("yp", [SEQ, D])
    ys = dt_out("ys", [16, D])
    kp = dt_out("kp", [DEPTH, SEQ, 384])
    vp = dt_out("vp", [DEPTH, SEQ, 384])
    ks = dt_out("ks", [DEPTH, 16, 384])
    vs = dt_out("vs", [DEPTH, 16, 384])
    rp = dt_out("rp", [DEPTH, NH, HD, HD])
    rs_o = dt_out("rs", [DEPTH, 4, NH, HD, HD])
    pp = dt_out("pp", [DEPTH, 15, 256])
    pso = dt_out("pso", [DEPTH, 4, 15, 256])
    y1s = nc.dram_tensor("y1s", [SEQ, D], F32, kind="Internal").ap()
    xs1 = nc.dram_tensor("xs1", [16, D], F32, kind="Internal").ap()

    with ExitStack() as st:
        S = Sched(nc, st)

        def sbt(stack, name, shape, dt):
            return stack.enter_context(nc.sbuf_tensor(name, shape, dt))

        PB = [st.enter_context(nc.psum_tensor("pb%d" % i, [128, 512], F32)) for i in range(8)]

        def pbf(i):
            return PB[i][:].bitcast(BF16)

        c16 = sbt(st, "c16s", [128, n16], BF16)
        c32 = sbt(st, "c32s", [128, n32], F32)
        win = sbt(st, "win", [128, 8, DIN], BF16)
        wout = sbt(st, "wout", [128, 8, D], BF16)
        nwb = sbt(st, "nwb", [128, D], F32)
        gnb = sbt(st, "gnb", [128, 384], F32)
        qkwb = sbt(st, "qkwb", [128, 2, HD], F32)
        psb = sbt(st, "psb", [128, 256], F32)
        pwa = sbt(st, "pwa", [65, 4, HD], BF16)

        def C16(name, rows=128):
            o, n = o16[name]
            return c16[0:rows, o:o + n]

        def C32(name, rows=128):
            o, n = o32[name]
            return c32[0:rows, o:o + n]

        S.dma("sp", lambda e: e.dma_start(out=c16[:], in_=c16d), writes=["c16"])
        S.dma("sp", lambda e: e.dma_start(out=c32[:], in_=c32d), writes=["c32"])

        ck_flat = ck.rearrange("l n p d -> (l n p) d")
        cv_flat = cv.rearrange("l n p d -> (l n p) d")

        def load_weights(l):
            ws = ExitStack()
            stg = [sbt(ws, "wstg%d_%d" % (l, i), [128, DIN], F32) for i in range(3)]
            stgo = [sbt(ws, "wsto%d_%d" % (l, i), [128, D], F32) for i in range(2)]
            segs_by_eng = (("dve", WIN_SEGS[0]), ("act", WIN_SEGS[2]), ("pool", WIN_SEGS[1]), ("pool", WIN_SEGS[3]),
                           ("pool", WIN_SEGS[4]), ("dve", WIN_SEGS[5]))
            for kc in range(8):
                bi = kc % 3
                S.dma("sp", lambda e, kc=kc, bi=bi: e.dma_start(out=stg[bi][:], in_=w_in[l, kc * 128:(kc + 1) * 128, :]),
                      writes=["wstg%d" % bi], lat=8.0)
                for eng, (so, sw, do) in segs_by_eng:
                    if eng == "act":
                        S.op("act", lambda e, kc=kc, bi=bi, so=so, sw=sw, do=do: e.copy(out=win[:, kc, do:do + sw], in_=stg[bi][:, so:so + sw]),
                             reads=["wstg%d" % bi], writes=["win_%d_%d" % (kc, do)], c=1.2)
                    else:
                        S.op(eng, lambda e, kc=kc, bi=bi, so=so, sw=sw, do=do: e.tensor_copy(out=win[:, kc, do:do + sw], in_=stg[bi][:, so:so + sw]),
                             reads=["wstg%d" % bi], writes=["win_%d_%d" % (kc, do)], c=(1.3 if eng == "dve" else 1.0))
            for kc in range(8):
                bi = kc % 2
                S.dma("sp", lambda e, kc=kc, bi=bi: e.dma_start(out=stgo[bi][:], in_=w_out[l, kc * 128:(kc + 1) * 128, :]),
                      writes=["wsto%d" % bi], lat=4.0)
                eng = ("dve", "act")[kc % 2]
                if eng == "act":
                    S.op("act", lambda e, kc=kc, bi=bi: e.copy(out=wout[:, kc, :], in_=stgo[bi][:]), reads=["wsto%d" % bi], writes=["wout_%d" % kc], c=1.0)
                else:
                    S.op("dve", lambda e, kc=kc, bi=bi: e.tensor_copy(out=wout[:, kc, :], in_=stgo[bi][:]), reads=["wsto%d" % bi], writes=["wout_%d" % kc], c=1.0)
            load_small(l)
            S.barrier()
            ws.close()

        def load_small(l):
            S.dma("sp", lambda e: e.dma_start(out=nwb[:], in_=norm_w[l:l + 1, :].to_broadcast([128, D])), writes=["nwb"])
            S.dma("sp", lambda e: e.dma_start(out=gnb[:], in_=gn_w[l:l + 1, :].to_broadcast([128, 384])), writes=["gnb"])
            S.dma("sp", lambda e: e.dma_start(out=qkwb[:, 0, :], in_=qn_w[l:l + 1, :].to_broadcast([128, HD])), writes=["qkwb"], key="dqkw")
            S.dma("sp", lambda e: e.dma_start(out=qkwb[:, 1, :], in_=kn_w[l:l + 1, :].to_broadcast([128, HD])), writes=["qkwb"], key="dqkw")
            S.dma("sp", lambda e: e.dma_start(out=psb[:], in_=pool_s[l:l + 1, :].to_broadcast([128, 256])), writes=["psb"])
            S.dma("pool", lambda e: e.dma_start(out=pwa[0:64, :, :], in_=pool_w[l].rearrange("g c d -> c g d")), writes=["pwa"], key="dpwa")
            S.dma("pool", lambda e: e.dma_start(out=pwa[64:65, :, :], in_=pool_b[l:l + 1, :, :]), writes=["pwa"], key="dpwa")

        def tt(eng, out, in0, in1, op, reads, writes):
            S.op(eng, lambda e: e.tensor_tensor(out=out, in0=in0, in1=in1, op=op), reads=reads, writes=writes)

        def ts(eng, out, in0, s1, s2, op0, op1, reads, writes):
            if op1 is None:
                S.op(eng, lambda e: e.tensor_scalar(out=out, in0=in0, scalar1=s1, scalar2=None, op0=op0), reads=reads, writes=writes)
            else:
                S.op(eng, lambda e: e.tensor_scalar(out=out, in0=in0, scalar1=s1, scalar2=s2, op0=op0, op1=op1), reads=reads, writes=writes)

        def rsq(out, in_, scale, bias, reads, writes):
            S.op("act", lambda e: e.activation(out=out, in_=in_, func=AF.Ln, scale=scale, bias=bias), reads=reads, writes=writes)
            S.op("act", lambda e: e.activation(out=out, in_=out, func=AF.Exp, scale=-0.5), reads=writes, writes=writes)

        def silu_gates(G, zs, tg):
            zg = zs[:, O_RG:O_RG + 1024]
            S.op("act", lambda e: e.activation(out=G[:], in_=zg, func=AF.Exp, scale=-1.0), reads=[tg + "zs"], writes=[tg + "G"])
            S.op("act", lambda e: e.activation(out=G[:], in_=G[:], func=AF.Ln, bias=1.0), reads=[tg + "G"], writes=[tg + "G"])
            S.op("act", lambda e: e.activation(out=G[:], in_=G[:], func=AF.Exp, scale=-1.0), reads=[tg + "G"], writes=[tg + "G"])
            tt("dve", G[:], G[:], zg, ALU.mult, [tg + "G", tg + "zs"], [tg + "G"])

        def rope(P, x768, cs, t1, t2, out768, tag, rd, okey=None):
            xv = x768.rearrange("p (h t d) -> p h t d", h=12, t=2)
            t1v = t1.rearrange("p (h t d) -> p h t d", h=12, t=2)
            t2v = t2.rearrange("p (h t d) -> p h t d", h=12, t=2)
            cosb = cs[:, 0:64].rearrange("p (t d) -> p t d", t=2).unsqueeze(1).to_broadcast([P, 12, 2, 32])
            nsin = cs[:, 64:96].unsqueeze(1).to_broadcast([P, 12, 32])
            psin = cs[:, 96:128].unsqueeze(1).to_broadcast([P, 12, 32])
            tk = tag[:-1]
            tt("dve", t1v, xv, cosb, ALU.mult, rd, [tk + "t1"])
            tt("dve", t2v[:, :, 0, :], xv[:, :, 1, :], nsin, ALU.mult, rd, [tk + "t2a"])
            tt("dve", t2v[:, :, 1, :], xv[:, :, 0, :], psin, ALU.mult, rd, [tk + "t2b"])
            tt("dve", out768, t1, t2, ALU.add, [tk + "t1", tk + "t2a", tk + "t2b"], [okey or (tag + "rot")])

        def sample_layer(l, kst):
            P = 16
            tg0 = "s%d_" % l
            kb_ = lambda name, shape, dt: sbt(kst, "k%d_" % l + name, shape, dt)
            mix = kb_("mix", [P, D], BF16)
            Gm = kb_("Gm", [P, 384], F32)
            vna = kb_("vna", [P, 6, 65], BF16)
            ptown = kb_("ptown", [P, 6, P], BF16)
            acc = kb_("acc", [P, 390], F32)
            Qblk = kb_("Qblk", [128, 3, 4, 2, 4], BF16)
            idxt = kb_("idxt", [128, 256], I32)
            NKB = 4
            kpg = [kb_("kpg%d" % i, [128, 384], BF16) for i in range(NKB)]
            kT = [kb_("kT%d" % i, [128, 3, 128], BF16) for i in range(2)]
            vau = [kb_("vau%d" % i, [128, 6, 65], BF16) for i in range(2)]
            Sall = kb_("Sall", [128, 64, 24], F32)
            PTp = kb_("PTp", [128, 16, 6, P], BF16)
            gate = kb_("gate", [24, 32], F32)
            m8 = kb_("m8", [24, 8], F32)
            negm = kb_("negm", [24, 32], F32)
            Dm = kb_("Dm", [24, 32, 24], BF16)
            ss = ExitStack()
            sb = lambda name, shape, dt: sbt(ss, "s%d_" % l + name, shape, dt)
            pts = sb("pts", [128, 256], I32)
            xss = sb("xss", [P, D], F32)
            xsrc = xs_d if l == 0 else xs1
            S.dma("sp", lambda e: e.dma_start(out=xss[:], in_=xsrc), reads=(["xs1"] if l > 0 else []), writes=[tg0 + "xss"])
            hb = sb("hb", [P, D], BF16)
            junk = sb("junk", [P, D], BF16)
            ssq = sb("ssq", [P, 4], F32)
            hT = sb("hT", [128, 8, P], BF16)
            zs = sb("zs", [P, DIN], F32)
            t1 = sb("t1", [P, 768], F32)
            t2 = sb("t2", [P, 768], F32)
            rot = sb("rot", [P, 768], F32)
            rqk = sb("rqk", [P, 768], BF16)
            rqkT = sb("rqkT", [64, 12, P], BF16)
            rvb = sb("rvb", [P, 384], BF16)
            kpad = sb("kpad", [P, 4, 384], BF16)
            qpad = sb("qpad", [64, 6, 4, P], BF16)
            S0 = sb("S0", [64, 4, NH, HD], F32)
            gS0 = sb("gS0", [64, 4, NH, HD], BF16)
            Sn = sb("Sn", [64, 4, NH, HD], F32)
            inT = sb("inT", [P, 6, P], BF16)
            osb = sb("osb", [P, 384], F32)
            osq = sb("osq", [P, 384], F32)
            st1 = sb("st1", [P, 6], F32)
            st2 = sb("st2", [P, 6], F32)
            st3 = sb("st3", [P, 6], F32)
            G = sb("G", [P, D], F32)
            mrot = sb("mrot", [P, 768], F32)
            mnorm = sb("mnorm", [P, 768], F32)
            mss = sb("mss", [P, 12], F32)
            mqkb = sb("mqkb", [P, 768], BF16)
            mqkT = sb("mqkT", [64, 12, P], BF16)
            qTp = sb("qTp", [128, 3, P], BF16)
            sown = sb("sown", [P, 6, P], F32)
            bufs = sb("bufs", [60, 256], F32)
            bufb = sb("bufb", [60, 256], BF16)
            ub = sb("ub", [P, 256], BF16)
            pT = sb("pT", [65, 4, P], BF16)
            pos = sb("pos", [P, 256], F32)
            tg = "s%d_" % l
            S.dma("sp", lambda e: e.dma_start(out=pts[:], in_=ptab.to_broadcast([128, 256])), writes=[tg + "pts"])
            S.op("dve", lambda e: e.tensor_scalar(out=idxt[:], in0=pts[:], scalar1=128.0, scalar2=C32("iota2")[:, l:l + 1], op0=ALU.mult, op1=ALU.add),
                 reads=[tg + "pts", "c32"], writes=[tg + "idxt"])
            cs = C32("rope_s", P)

            S.dma("sp", lambda e: e.dma_start(out=S0[:], in_=sret[l].rearrange("b h d e -> d b h e")), writes=[tg + "S0"])
            S.dma("sp", lambda e: e.dma_start(out=bufs[:], in_=spool[l]), writes=[tg + "bufs"])
            S.op("pool", lambda e: e.tensor_copy(out=bufb[:], in_=bufs[:]), reads=[tg + "bufs"], writes=[tg + "bufb"])
            for b in range(4):
                S.dma("sp", lambda e, b=b: e.dma_start(out=pso[l, b, 0:11, :], in_=bufs[15 * b + 4:15 * b + 15, :]),
                      reads=[tg + "bufs"], key="o_pso")
            S.op("act", lambda e: e.activation(out=junk[:], in_=xss[:], func=AF.Square, accum_out=ssq[:, 0:1]),
                 reads=[tg + "xss"], writes=[tg + "ssq", tg + "junk"])
            rsq(ssq[:, 2:3], ssq[:, 0:1], 1.0 / D, EPS, [tg + "ssq"], [tg + "rstd"])
            rstd = ssq[:, 2:3]
            tt("dve", hb[:], xss[:], nwb[0:P, :], ALU.mult, [tg + "xss", "nwb"], [tg + "hb"])
            for kc in range(8):
                S.op("pe", lambda e, kc=kc: e.transpose(out=pbf(2)[:, kc * P:(kc + 1) * P], in_=hb[:, kc * 128:(kc + 1) * 128], identity=C16("ident", P)[:, 0:P]),
                     reads=[tg + "hb", "c16"], writes=["pb2"], mark=(kc == 7))
            S.op("dve", lambda e: e.tensor_copy(out=hT[:].rearrange("p k t -> p (k t)"), in_=pbf(2)[:, 0:8 * P]), reads=["pb2"], writes=[tg + "hT"])
            for n in range(7):
                bk = n % 2
                for kc in range(8):
                    S.op("pe", lambda e, kc=kc, n=n, bk=bk: e.matmul(PB[bk][0:P, :], lhsT=hT[:, kc, :], rhs=win[:, kc, n * 512:(n + 1) * 512], start=(kc == 0), stop=(kc == 7)),
                         reads=[tg + "hT", "win"], writes=["pb%d" % bk], mark=(kc == 7))
                S.op("act", lambda e, n=n, bk=bk: e.activation(out=zs[:, n * 512:(n + 1) * 512], in_=PB[bk][0:P, :], func=AF.Copy, scale=rstd),
                     reads=["pb%d" % bk, tg + "rstd"], writes=[tg + "zs"])
            S.dma("sp", lambda e: e.dma_start(out=vs[l], in_=zs[:, O_MV:O_MV + 384]), reads=[tg + "zs"], key="o_vs")
            S.dma("sp", lambda e: e.dma_start(out=pso[l, :, 11:15, :].rearrange("b q c -> (b q) c") if False else pso[l, 0, 11:15, :], in_=zs[0:4, O_PU:O_PU + 256]), reads=[tg + "zs"], key="o_pso")
            for b in range(1, 4):
                S.dma("sp", lambda e, b=b: e.dma_start(out=pso[l, b, 11:15, :], in_=zs[4 * b:4 * b + 4, O_PU:O_PU + 256]), reads=[tg + "zs"], key="o_pso")
            silu_gates(G, zs, tg)
            S.op("act", lambda e: e.copy(out=Gm[:], in_=G[:, 384:768]), reads=[tg + "G"], writes=[tg + "Gm"])
            rope(P, zs[:, 0:768], cs, t1[:], t2[:], rot[:], tg + "r", [tg + "zs", "c32"])
            tt("dve", rqk[:].rearrange("p (h d) -> p h d", h=12), rot[:].rearrange("p (h d) -> p h d", h=12),
               C32("gqk_s", P).unsqueeze(2).to_broadcast([P, 12, 64]), ALU.mult, [tg + "rrot", "c32"], [tg + "rqk"])
            S.op("pool", lambda e: e.tensor_copy(out=rvb[:], in_=zs[:, O_RV:O_RV + 384]), reads=[tg + "zs"], writes=[tg + "rvb"])
            for h in range(12):
                S.op("pe", lambda e, h=h: e.transpose(out=pbf(3)[0:64, h * P:(h + 1) * P], in_=rqk[:, h * 64:(h + 1) * 64], identity=C16("ident", P)[:, 0:P]),
                     reads=[tg + "rqk", "c16"], writes=["pb3"], mark=(h == 11))
            S.op("dve", lambda e: e.tensor_copy(out=rqkT[:].rearrange("p h t -> p (h t)"), in_=pbf(3)[0:64, 0:12 * P]), reads=["pb3"], writes=[tg + "rqkT"])
            tt("dve", qpad[:], rqkT[:, 0:6, :].unsqueeze(2).to_broadcast([64, 6, 4, P]),
               C32("bmask", 64).rearrange("p (b t) -> p b t", b=4).unsqueeze(1).to_broadcast([64, 6, 4, P]), ALU.mult,
               [tg + "rqkT", "c32"], [tg + "qpad"])
            tt("dve", kpad[:], rqk[:, 384:768].unsqueeze(1).to_broadcast([P, 4, 384]),
               C32("rowmask", P).unsqueeze(2).to_broadcast([P, 4, 384]), ALU.mult, [tg + "rqk", "c32"], [tg + "kpad"])
            tt("dve", gS0[:], S0[:], C32("dec", 64)[:, 12:18].unsqueeze(1).unsqueeze(3).to_broadcast([64, 4, NH, HD]), ALU.mult,
               [tg + "S0", "c32"], [tg + "gS0"])
            for h in range(6):
                S.op("pe", lambda e, h=h: e.matmul(PB[4][0:P, h * P:(h + 1) * P], lhsT=rqkT[:, 6 + h, :], rhs=rqkT[:, h, :], start=True, stop=True),
                     reads=[tg + "rqkT"], writes=["pb4"], mark=(h == 5))
            tt("dve", inT[:], PB[4][0:P, 0:6 * P].rearrange("p (h t) -> p h t", h=6),
               C32("mask01s", P).unsqueeze(1).to_broadcast([P, 6, P]), ALU.mult, ["pb4", "c32"], [tg + "inT"])
            for h in range(6):
                S.op("pe", lambda e, h=h: e.matmul(PB[5][0:P, h * 64:(h + 1) * 64], lhsT=inT[:, h, :], rhs=rvb[:, h * 64:(h + 1) * 64], start=True, stop=False),
                     reads=[tg + "inT", tg + "rvb"], writes=["pb5"], mark=False)
                for b in range(4):
                    S.op("pe", lambda e, h=h, b=b: e.matmul(PB[5][0:P, h * 64:(h + 1) * 64], lhsT=qpad[:, h, b, :], rhs=gS0[:, b, h, :], start=False, stop=(b == 3)),
                         reads=[tg + "qpad", tg + "gS0"], writes=["pb5"], mark=(b == 3 and h == 5))
            for b in range(4):
                for h in range(6):
                    S.op("pe", lambda e, h=h, b=b: e.matmul(PB[4][0:64, h * 64:(h + 1) * 64], lhsT=kpad[:, b, h * 64:(h + 1) * 64], rhs=rvb[:, h * 64:(h + 1) * 64], start=True, stop=True),
                         reads=[tg + "kpad", tg + "rvb"], writes=["pb4"], mark=(h == 5))
                tt("dve", Sn[:, b, :, :], PB[4][0:64, 0:384].rearrange("p (h e) -> p h e", h=6),
                   C32("dec", 64)[:, 24:30].unsqueeze(2).to_broadcast([64, NH, HD]), ALU.mult, ["pb4", "c32"], [tg + "Sn"])
                tt("pool", S0[:, b, :, :], S0[:, b, :, :], C32("dec", 64)[:, 18:24].unsqueeze(2).to_broadcast([64, NH, HD]), ALU.mult,
                   [tg + "gS0", "c32"], [tg + "S0"])
                tt("dve", Sn[:, b, :, :], Sn[:, b, :, :], S0[:, b, :, :], ALU.add, [tg + "Sn", tg + "S0"], [tg + "Sn"])
            S.dma("sp", lambda e: e.dma_start(out=rs_o[l].rearrange("b h d e -> d b h e"), in_=Sn[:]), reads=[tg + "Sn"], key="o_rs")
            group_norm(P, PB[5][0:P, 0:384], "pb5", osb, osq, st1, st2, st3, G[:, 0:384], mix[:, 0:384], tg)
            S.op("pool", lambda e: e.tensor_copy(out=ub[:], in_=zs[:, O_PU:O_PU + 256]), reads=[tg + "zs"], writes=[tg + "ub"])
            for g in range(4):
                S.op("pe", lambda e, g=g: e.matmul(PB[4][0:64, g * P:(g + 1) * P], lhsT=bufb[:, g * 64:(g + 1) * 64], rhs=C16("Ab", 60)[:, g * 16:(g + 1) * 16], start=True, stop=False),
                     reads=[tg + "bufb", "c16"], writes=["pb4"], mark=False)
                S.op("pe", lambda e, g=g: e.matmul(PB[4][0:64, g * P:(g + 1) * P], lhsT=ub[:, g * 64:(g + 1) * 64], rhs=C16("Ac", P)[:, g * 16:(g + 1) * 16], start=False, stop=True),
                     reads=[tg + "ub", "c16"], writes=["pb4"], mark=(g == 3))
            S.op("pool", lambda e: e.memset(pT[64:65, :, :], 1.0), writes=[tg + "pT1"])
            S.op("dve", lambda e: e.tensor_copy(out=pT[0:64, :, :].rearrange("p g t -> p (g t)"), in_=PB[4][0:64, 0:4 * P]), reads=["pb4"], writes=[tg + "pT"])
            for g in range(4):
                S.op("pe", lambda e, g=g: e.matmul(PB[5][0:P, g * 64:(g + 1) * 64], lhsT=pT[0:65, g, :], rhs=pwa[0:65, g, :], start=True, stop=True),
                     reads=[tg + "pT", tg + "pT1", "pwa"], writes=["pb5"], mark=(g == 3))
            tt("dve", pos[:], PB[5][0:P, 0:256], psb[0:P, :], ALU.mult, ["pb5", "psb"], [tg + "pos"])
            tt("dve", mix[:, 768:1024], pos[:], G[:, 768:1024], ALU.mult, [tg + "pos", tg + "G"], [tg + "mixp"])
            qk_norm(P, zs[:, O_MQ:O_MQ + 768], tg + "zs", mnorm, osq_big=t1, mss=mss, tag=tg)
            rope(P, mnorm[:], cs, t1[:], t2[:], mrot[:], tg + "m", [tg + "mnorm", "c32"])
            S.dma("sp", lambda e: e.dma_start(out=ks[l], in_=mrot[:, 384:768]), reads=[tg + "mrot"], key="o_ks")
            S.op("pool", lambda e: e.tensor_copy(out=mqkb[:], in_=mrot[:]), reads=[tg + "mrot"], writes=[tg + "mqkb"])
            S.op("pool", lambda e: e.memset(vna[:, :, 64:65], 1.0), writes=[tg + "vna1"])
            S.op("pool", lambda e: e.tensor_copy(out=vna[:, :, 0:64], in_=zs[:, O_MV:O_MV + 384].rearrange("p (h d) -> p h d", h=6)), reads=[tg + "zs"], writes=[tg + "vna"])
            for h in range(12):
                S.op("pe", lambda e, h=h: e.transpose(out=pbf(3)[0:64, h * P:(h + 1) * P], in_=mqkb[:, h * 64:(h + 1) * 64], identity=C16("ident", P)[:, 0:P]),
                     reads=[tg + "mqkb", "c16"], writes=["pb3"], mark=(h == 11))
            S.op("dve", lambda e: e.tensor_copy(out=mqkT[:].rearrange("p h t -> p (h t)"), in_=pbf(3)[0:64, 0:12 * P]), reads=["pb3"], writes=[tg + "mqkT"])
            for c in range(3):
                S.op("pe", lambda e, c=c: e.transpose(out=pbf(2)[:, c * P:(c + 1) * P], in_=mqkb[:, c * 128:(c + 1) * 128], identity=C16("ident", P)[:, 0:P]),
                     reads=[tg + "mqkb", "c16"], writes=["pb2"], mark=(c == 2))
            S.op("dve", lambda e: e.tensor_copy(out=qTp[:].rearrange("p c t -> p (c t)"), in_=pbf(2)[:, 0:3 * P]), reads=["pb2"], writes=[tg + "qTp"])
            S.op("pool", lambda e: e.memset(Qblk[:].rearrange("p c b s q -> p (c b s q)"), 0.0), writes=[tg + "Qblk"])
            for hp in range(2):
                S.op("dve", lambda e, hp=hp: e.tensor_copy(out=Qblk[hp * 64:(hp + 1) * 64, :, :, hp, :],
                                                          in_=qTp[hp * 64:(hp + 1) * 64, :, :].rearrange("p c (b q) -> p c b q", b=4)),
                     reads=[tg + "qTp", tg + "Qblk"], writes=[tg + "Qblk"])
            for h in range(6):
                S.op("pe", lambda e, h=h: e.matmul(PB[4][0:P, h * P:(h + 1) * P], lhsT=mqkT[:, 6 + h, :], rhs=mqkT[:, h, :], start=True, stop=True),
                     reads=[tg + "mqkT"], writes=["pb4"], mark=(h == 5))
            tt("dve", sown[:], PB[4][0:P, 0:6 * P].rearrange("p (h t) -> p h t", h=6),
               C32("ownmask", P).unsqueeze(1).to_broadcast([P, 6, P]), ALU.add, ["pb4", "c32"], [tg + "sown"])
            S.op("act", lambda e: e.activation(out=ptown[:], in_=sown[:], func=AF.Exp, scale=0.125), reads=[tg + "sown"], writes=[tg + "ptown"])

            S.barrier()
            ss.close()
            yield "pre"
            for i in range(2):
                S.op("pool", lambda e, i=i: e.memset(vau[i][:, :, 64:65], 1.0), writes=[tg + "vau1_%d" % i])
            kcnt = [0]

            def page_dma(cache, b, pg, slot):
                col = b * 64 + pg
                S.dma("pool", lambda e: e.indirect_dma_start(out=kpg[slot][:], out_offset=None, in_=cache,
                                                             in_offset=bass.IndirectOffsetOnAxis(ap=idxt[:, col:col + 1], axis=0)),
                      reads=[tg + "idxt"], writes=[tg + "kpg%d" % slot])

            for b in range(4):
                for pg in range(NPAGES):
                    n = kcnt[0]
                    kcnt[0] += 1
                    slot = n % NKB
                    s2 = n % 2
                    page_dma(ck_flat, b, pg, slot)
                    for c in range(3):
                        S.op("pe", lambda e, c=c, slot=slot: e.transpose(out=pbf(7)[:, c * 128:(c + 1) * 128], in_=kpg[slot][:, c * 128:(c + 1) * 128], identity=C16("ident")),
                             reads=[tg + "kpg%d" % slot, "c16"], writes=["pb7"], mark=(c == 2))
                    if pg % 2 == 0:
                        S.op("dve", lambda e, s2=s2: e.tensor_copy(out=kT[s2][:].rearrange("p c k -> p (c k)"), in_=pbf(7)[:, 0:384]),
                             reads=["pb7"], writes=[tg + "kT%d" % s2])
                    else:
                        S.op("act", lambda e, s2=s2: e.copy(out=kT[s2][:].rearrange("p c k -> p (c k)"), in_=pbf(7)[:, 0:384]),
                             reads=["pb7"], writes=[tg + "kT%d" % s2])
                    pl = pg % 8
                    for c in range(3):
                        S.op("pe", lambda e, c=c, s2=s2, pl=pl, b=b: e.matmul(PB[7][:, 256 + pl * 24 + c * 8:256 + pl * 24 + c * 8 + 8], lhsT=kT[s2][:, c, :],
                                                                            rhs=Qblk[:, c, b, :, :].rearrange("p s q -> p (s q)"), start=True, stop=True),
                             reads=[tg + "kT%d" % s2, tg + "Qblk"], writes=["pb7"], mark=(c == 2))
                    if pl == 7:
                        p0 = pg - 7
                        S.op("act", lambda e, p0=p0: e.copy(out=Sall[:, p0:p0 + 8, :].rearrange("p a b -> p (a b)"), in_=PB[7][:, 256:448]),
                             reads=["pb7"], writes=[tg + "Sall"])
                    yield "page"
                for pg in range(NPAGES):
                    S.op("pe", lambda e, pg=pg: e.matmul(PB[7][0:24, 448 + pg // 2:448 + pg // 2 + 1], lhsT=Sall[:, pg, :], rhs=C32("ones")[:, 0:1], start=(pg % 2 == 0), stop=(pg % 2 == 1)),
                         reads=[tg + "Sall", "c32"], writes=["pb7"], mark=(pg == NPAGES - 1))
                S.op("dve", lambda e: e.tensor_copy(out=gate[:], in_=PB[7][0:24, 448:480]), reads=["pb7"], writes=[tg + "gate"])
                S.op("dve", lambda e: e.max(out=m8[:], in_=gate[:]), reads=[tg + "gate"], writes=[tg + "m8"])
                tt("dve", negm[:], gate[:], m8[:, 2:3].to_broadcast([24, 32]), ALU.is_ge, [tg + "gate", tg + "m8"], [tg + "negm"])
                ts("dve", negm[:], negm[:], -1.0, -NEG, ALU.add, ALU.mult, [tg + "negm"], [tg + "negm"])
                tt("dve", Dm[:], negm[:].unsqueeze(2).to_broadcast([24, 32, 24]),
                   C32("ident24", 24).unsqueeze(1).to_broadcast([24, 32, 24]), ALU.mult, [tg + "negm", "c32"], [tg + "Dm"])
                yield "gate"
                for hh in range(2):
                    S.op("pe", lambda e, hh=hh: e.matmul(PB[7][:, 0:384], lhsT=C16("ones", 24), rhs=Dm[:, hh * 16:(hh + 1) * 16, :].rearrange("p a b -> p (a b)"), start=True, stop=True),
                         reads=[tg + "Dm", "c16"], writes=["pb7"])
                    tt("dve", Sall[:, hh * 32:(hh + 1) * 32, :].rearrange("p (k t) c -> p k t c", t=2),
                       Sall[:, hh * 32:(hh + 1) * 32, :].rearrange("p (k t) c -> p k t c", t=2),
                       PB[7][:, 0:384].rearrange("p (k c) -> p k c", k=16).unsqueeze(2).to_broadcast([128, 16, 2, 24]), ALU.add,
                       [tg + "Sall", "pb7"], [tg + "Sall"])
                    yield "mask"
                S.op("dve", lambda e: e.memset(PTp[:].rearrange("p a h t -> p (a h t)"), 0.0), writes=[tg + "PTp"])
                for pg in range(NPAGES):
                    if pg % 16 == 0:
                        S.op("act", lambda e, b=b, pg=pg: e.activation(out=PTp[:, :, :, 4 * b:4 * b + 4], in_=Sall[:, pg:pg + 16, :].rearrange("p a (h q) -> p a h q", h=6), func=AF.Exp, scale=0.125),
                             reads=[tg + "Sall"], writes=[tg + "PTp"])
                    n = kcnt[0]
                    kcnt[0] += 1
                    slot = n % NKB
                    s2 = n % 2
                    page_dma(cv_flat, b, pg, slot)
                    S.op("act", lambda e, slot=slot, s2=s2: e.copy(out=vau[s2][:, :, 0:64], in_=kpg[slot][:].rearrange("p (h d) -> p h d", h=6)),
                         reads=[tg + "kpg%d" % slot], writes=[tg + "vau%d" % s2])
                    S.op("pe", lambda e, pg=pg, s2=s2: e.matmul(PB[7][0:96, 0:390], lhsT=PTp[:, pg % 16, :, :].rearrange("p h t -> p (h t)"),
                                                              rhs=vau[s2][:].rearrange("p h e -> p (h e)"), start=(pg == 0), stop=(pg == NPAGES - 1)),
                         reads=[tg + "PTp", tg + "vau%d" % s2, tg + "vau1_%d" % s2], writes=["pb7"], c=0.22)
                    yield "page"
                msk = Sall[0:96, 0:17, :].rearrange("p a b -> p (a b)")[:, 0:390]
                tt("dve", msk, PB[7][0:96, 0:390], C16("dmask", 96), ALU.mult, ["pb7", "c16"], [tg + "Sall"])
                S.op("pe", lambda e: e.matmul(PB[7][0:P, 0:390], lhsT=C32("sel96", 96), rhs=msk, start=True, stop=True),
                     reads=[tg + "Sall", "c32"], writes=["pb7"], c=0.8)
                if b == 0:
                    S.op("dve", lambda e: e.tensor_copy(out=acc[:], in_=PB[7][0:P, 0:390]), reads=["pb7"], writes=[tg + "acc"])
                else:
                    tt("dve", acc[:], acc[:], PB[7][0:P, 0:390], ALU.add, [tg + "acc", "pb7"], [tg + "acc"])
            yield "pages"
            ss = ExitStack()
            sb = lambda name, shape, dt: sbt(ss, "t%d_" % l + name, shape, dt)
            mixT = sb("mixT", [128, 8, P], BF16)
            xs2 = sb("xs2", [P, D], F32)
            S.dma("sp", lambda e: e.dma_start(out=xs2[:], in_=xsrc), writes=[tg + "xs2"])
            mo = sb("mo", [P, 6, 64], F32)
            rden = sb("rden", [P, 6], F32)
            for h in range(6):
                S.op("pe", lambda e, h=h: e.matmul(PB[3][0:P, h * 65:(h + 1) * 65], lhsT=ptown[:, h, :], rhs=vna[:, h, :], start=True, stop=True),
                     reads=[tg + "ptown", tg + "vna", tg + "vna1"], writes=["pb3"], mark=(h == 5))
            tt("dve", acc[:], acc[:], PB[3][0:P, 0:390], ALU.add, [tg + "acc", "pb3"], [tg + "acc"])
            pvv = acc[:].rearrange("p (h e) -> p h e", h=6)
            S.op("dve", lambda e: e.reciprocal(out=rden[:].unsqueeze(2), in_=pvv[:, :, 64:65]), reads=[tg + "acc"], writes=[tg + "rden"])
            tt("dve", mo[:], pvv[:, :, 0:64], rden[:].unsqueeze(2).to_broadcast([P, 6, 64]), ALU.mult, [tg + "acc", tg + "rden"], [tg + "mo"])
            tt("dve", mix[:, 384:768], mo[:].rearrange("p h e -> p (h e)"), Gm[:], ALU.mult, [tg + "mo", tg + "Gm"], [tg + "mixm"])
            for kc in range(8):
                S.op("pe", lambda e, kc=kc: e.transpose(out=pbf(2)[:, kc * P:(kc + 1) * P], in_=mix[:, kc * 128:(kc + 1) * 128], identity=C16("ident", P)[:, 0:P]),
                     reads=[tg + "mixr", tg + "mixm", tg + "mixp", "c16"], writes=["pb2"], mark=(kc == 7))
            S.op("dve", lambda e: e.tensor_copy(out=mixT[:].rearrange("p k t -> p (k t)"), in_=pbf(2)[:, 0:8 * P]), reads=["pb2"], writes=[tg + "mixT"])
            for n in range(2):
                for kc in range(8):
                    S.op("pe", lambda e, kc=kc, n=n: e.matmul(PB[n][0:P, :], lhsT=mixT[:, kc, :], rhs=wout[:, kc, n * 512:(n + 1) * 512], start=(kc == 0), stop=(kc == 7)),
                         reads=[tg + "mixT", "wout"], writes=["pb%d" % n], mark=(kc == 7))
                tt("dve", xs2[:, n * 512:(n + 1) * 512], PB[n][0:P, :], xs2[:, n * 512:(n + 1) * 512], ALU.add, ["pb%d" % n, tg + "xs2"], [tg + "xs2"])
            if l == DEPTH - 1:
                S.dma("sp", lambda e: e.dma_start(out=ys, in_=xs2[:]), reads=[tg + "xs2"], key="o_ys")
            else:
                S.dma("sp", lambda e: e.dma_start(out=xs1, in_=xs2[:]), reads=[tg + "xs2"], writes=["xs1"], key="dxs1")

            S.barrier()
            ss.close()
            yield "post"

        def group_norm(P, o_ps, o_key, osb, osq, st1, st2, st3, Gs, out_mix, tg, gkeys=None):
            S.op("act", lambda e: e.copy(out=osb[:], in_=o_ps), reads=[o_key], writes=[tg + "osb"])
            ov = osb[:].rearrange("p (h d) -> p h d", h=6)
            S.op("dve", lambda e: e.tensor_reduce(out=st1[:], in_=ov, axis=AX.X, op=ALU.add), reads=[tg + "osb"], writes=[tg + "st1"])
            ts("dve", st1[:], st1[:], 1.0 / 64, None, ALU.mult, None, [tg + "st1"], [tg + "st1"])
            tt("dve", ov, ov, st1[:].unsqueeze(2).to_broadcast([P, 6, 64]), ALU.subtract, [tg + "osb", tg + "st1"], [tg + "osb"])
            tt("dve", osq[:], osb[:], osb[:], ALU.mult, [tg + "osb"], [tg + "osq"])
            S.op("dve", lambda e: e.tensor_reduce(out=st2[:], in_=osq[:].rearrange("p (h d) -> p h d", h=6), axis=AX.X, op=ALU.add), reads=[tg + "osq"], writes=[tg + "st2"])
            rsq(st3[:], st2[:], 1.0 / 64, EPS, [tg + "st2"], [tg + "st3"])
            tt("dve", ov, ov, st3[:].unsqueeze(2).to_broadcast([P, 6, 64]), ALU.mult, [tg + "osb", tg + "st3"], [tg + "osb"])
            tt("dve", osb[:], osb[:], gnb[0:P, :], ALU.mult, [tg + "osb", "gnb"], [tg + "osb"])
            tt("dve", out_mix, osb[:], Gs, ALU.mult, [tg + "osb"] + (gkeys or [tg + "G"]), [tg + "mixr"])

        def qk_norm(P, x768, xkey, mnorm, osq_big, mss, tag):
            S.op("act", lambda e: e.activation(out=osq_big[:], in_=x768, func=AF.Square), reads=[xkey], writes=[tag + "t1"])
            S.op("dve", lambda e: e.tensor_reduce(out=mss[:], in_=osq_big[:].rearrange("p (h d) -> p h d", h=12), axis=AX.X, op=ALU.add),
                 reads=[tag + "t1"], writes=[tag + "mss"])
            rsq(mss[:], mss[:], 1.0 / 64, EPS, [tag + "mss"], [tag + "mss"])
            mn = x768 if mnorm is None else mnorm[:]
            mk_ = xkey if mnorm is None else tag + "mnorm"
            tt("dve", mn.rearrange("p (h d) -> p h d", h=12), x768.rearrange("p (h d) -> p h d", h=12),
               mss[:].unsqueeze(2).to_broadcast([P, 12, 64]), ALU.mult, [xkey, tag + "mss"], [mk_])
            tt("dve", mn.rearrange("p (s h d) -> p s h d", s=2, h=6), mn.rearrange("p (s h d) -> p s h d", s=2, h=6),
               qkwb[0:P, :, :].unsqueeze(2).to_broadcast([P, 2, 6, 64]), ALU.mult, [mk_, "qkwb"], [mk_])

        def prompt_layer(l, ps_, pump):
            P = 128
            sb = lambda name, shape, dt: sbt(ps_, "p%d_" % l + name, shape, dt)
            xt = [sb("xt%d" % i, [P, D], F32) for i in range(2)]
            cst = [sb("cs%d" % i, [P, 128], F32) for i in range(2)]
            hb = sb("hb", [P, D], BF16)
            ssq = sb("ssq", [P, 4], F32)
            hT = sb("hT", [128, 8, P], BF16)
            zs = sb("zs", [P, O_RG], F32)
            t1 = sb("t1", [P, 768], F32)
            t2 = sb("t2", [P, 768], F32)
            rot = sb("rot", [P, 768], F32)
            rqk = sb("rqk", [P, 768], BF16)
            rqkT = sb("rqkT", [64, 12, P], BF16)
            rvb = sb("rvb", [P, 384], BF16)
            Sst = sb("Sst", [64, NH, HD], F32)
            Stm = sb("Stm", [64, NH, HD], F32)
            gS = sb("gS", [64, NH, HD], BF16)
            inT = sb("inT", [P, 6, P], BF16)
            osb = sb("osb", [P, 384], F32)
            osq = sb("osq", [P, 384], F32)
            st1 = sb("st1", [P, 6], F32)
            st2 = sb("st2", [P, 6], F32)
            st3 = sb("st3", [P, 6], F32)
            G = sb("G", [P, D], F32)
            mix = sb("mix", [P, D], BF16)
            mss = sb("mss", [P, 12], F32)
            qaug = sb("qaug", [P, 6, 72], BF16)
            kb = sb("kb", [P, 384], BF16)
            qTa = sb("qTa", [72, 6, P], BF16)
            kTa = sb("kTa", [72, 6, SEQ], BF16)
            vaug = sb("vaug", [P, NT, 6, 65], BF16)
            kmf = sb("kmf", [64, 6, 8], F32)
            kmT = sb("kmT", [64, 6, 8], BF16)
            gsb = sb("gsb", [P, 6, 8], F32)
            m8 = sb("m8", [P, 6, 8], F32)
            sel = sb("sel", [P, 6, 8], F32)
            PT = [sb("PT%d" % i, [P, 4, P], BF16) for i in range(2)]
            rden = sb("rden", [P, 6], F32)
            ub = [sb("ub%d" % i, [P, 256], BF16) for i in range(2)]
            pT = sb("pT", [65, 4, P], BF16)
            pos = sb("pos", [P, 256], F32)
            tg = "p%d_" % l

            S.op("pool", lambda e: e.memset(Sst[:].rearrange("p h e -> p (h e)"), 0.0), writes=[tg + "S"])
            S.op("pool", lambda e: e.memset(gS[:].rearrange("p h e -> p (h e)"), 0.0), writes=[tg + "gS"])
            S.op("pool", lambda e: e.memset(vaug[:, :, :, 64:65], 1.0), writes=[tg + "vaug1"])
            S.op("pool", lambda e: e.memset(pT[64:65, :, :], 1.0), writes=[tg + "pT1"])
            for h in range(6):
                S.dma("sp", lambda e, h=h: e.dma_start(out=kTa[64:72, h, :], in_=blkd),
                      writes=[tg + "kTaI"], key="dkTaI")
            S.op("pool", lambda e: e.memset(qaug[:, :, 64:72], 0.0), writes=[tg + "qaugm"])

            def load_x(t):
                src = xp if l == 0 else y1s
                S.dma("sp", lambda e: e.dma_start(out=xt[t % 2][:], in_=src[t * 128:(t + 1) * 128, :]),
                      reads=(["y1s"] if l > 0 else []), writes=[tg + "xt%d" % (t % 2)])
                S.dma("sp", lambda e: e.dma_start(out=cst[t % 2][:], in_=roped[t * 128:(t + 1) * 128, :]), writes=[tg + "cs%d" % (t % 2)])

            load_x(0)
            for t in range(NT):
                xb = xt[t % 2]
                xk = tg + "xt%d" % (t % 2)
                csb = cst[t % 2]
                ck_ = tg + "cs%d" % (t % 2)
                S.op("act", lambda e, xb=xb: e.activation(out=hb[:], in_=xb[:], func=AF.Square, accum_out=ssq[:, 0:1]),
                     reads=[xk], writes=[tg + "ssq", tg + "hb"])
                rsq(ssq[:, 2:3], ssq[:, 0:1], 1.0 / D, EPS, [tg + "ssq"], [tg + "rstd"])
                rstd = ssq[:, 2:3]
                nrstd = ssq[:, 3:4]
                ts("dve", nrstd, rstd, -1.0, None, ALU.mult, None, [tg + "rstd"], [tg + "rstd"])
                tt("dve", hb[:], xb[:], nwb[:], ALU.mult, [xk, "nwb"], [tg + "hb"])
                for kc in range(8):
                    S.op("pe", lambda e, kc=kc: e.transpose(out=pbf(2)[:, kc * 128:(kc + 1) * 128], in_=hb[:, kc * 128:(kc + 1) * 128], identity=C16("ident")),
                         reads=[tg + "hb", "c16"], writes=["pb2"], mark=(kc == 7))
                S.op("act", lambda e: e.copy(out=hT[:].rearrange("p k t -> p (k t)"), in_=pbf(2)[:, 0:1024]), reads=["pb2"], writes=[tg + "hT"])
                for n in range(7):
                    bk = n % 2
                    for kc in range(8):
                        S.op("pe", lambda e, kc=kc, n=n, bk=bk: e.matmul(PB[bk][:, :], lhsT=hT[:, kc, :], rhs=win[:, kc, n * 512:(n + 1) * 512], start=(kc == 0), stop=(kc == 7)),
                             reads=[tg + "hT", "win"], writes=["pb%d" % bk], mark=(kc == 7), c=0.216)
                    if n < 5:
                        S.op("act", lambda e, n=n, bk=bk: e.activation(out=zs[:, n * 512:(n + 1) * 512], in_=PB[bk][:, :], func=AF.Copy, scale=rstd),
                             reads=["pb%d" % bk, tg + "rstd"], writes=[tg + "zs"], c=0.8)
                    else:
                        gs = G[:, (n - 5) * 512:(n - 4) * 512]
                        gk = tg + "G%d" % (n - 5)
                        S.op("act", lambda e, gs=gs, bk=bk: e.activation(out=gs, in_=PB[bk][:, :], func=AF.Exp, scale=nrstd),
                             reads=["pb%d" % bk, tg + "rstd"], writes=[gk])
                        S.op("act", lambda e, gs=gs: e.activation(out=gs, in_=gs, func=AF.Ln, bias=1.0), reads=[gk], writes=[gk])
                        S.op("act", lambda e, gs=gs: e.activation(out=gs, in_=gs, func=AF.Exp, scale=-1.0), reads=[gk], writes=[gk])
                        S.op("dve", lambda e, gs=gs, bk=bk: e.scalar_tensor_tensor(out=gs, in0=PB[bk][:, :], scalar=rstd, in1=gs, op0=ALU.mult, op1=ALU.mult),
                             reads=["pb%d" % bk, tg + "rstd", gk], writes=[gk])
                    pump(1)
                if t + 1 < NT:
                    load_x(t + 1)
                S.dma("sp", lambda e, t=t: e.dma_start(out=vp[l, t * 128:(t + 1) * 128, :], in_=zs[:, O_MV:O_MV + 384]), reads=[tg + "zs"], key="o_vp")
                if t == NT - 1:
                    S.dma("sp", lambda e: e.dma_start(out=pp[l], in_=zs[113:128, O_PU:O_PU + 256]), reads=[tg + "zs"], key="o_pp")
                pump(2)
                rope(P, zs[:, 0:768], csb, t1[:], t2[:], rot[:], tg + "r", [tg + "zs", ck_], okey=tg + "rot")
                tt("dve", rqk[:].rearrange("p (h d) -> p h d", h=12), rot[:].rearrange("p (h d) -> p h d", h=12),
                   C32("gqk").unsqueeze(2).to_broadcast([P, 12, 64]), ALU.mult, [tg + "rot", "c32"], [tg + "rqk"])
                S.op("act", lambda e: e.copy(out=rvb[:], in_=zs[:, O_RV:O_RV + 384]), reads=[tg + "zs"], writes=[tg + "rvb"])
                for half in range(2):
                    for hh in range(6):
                        h = half * 6 + hh
                        S.op("pe", lambda e, h=h, hh=hh: e.transpose(out=pbf(2)[0:64, hh * 128:(hh + 1) * 128], in_=rqk[:, h * 64:(h + 1) * 64], identity=C16("ident")),
                             reads=[tg + "rqk", "c16"], writes=["pb2"], mark=(hh == 5))
                    S.op("act", lambda e, half=half: e.copy(out=rqkT[:, half * 6:(half + 1) * 6, :].rearrange("p h t -> p (h t)"), in_=pbf(2)[0:64, 0:768]),
                         reads=["pb2"], writes=[tg + "rqkT"])
                pump(2)
                for hg in range(2):
                    for hh in range(3):
                        h = hg * 3 + hh
                        S.op("pe", lambda e, h=h, hh=hh: e.matmul(PB[4][:, hh * 128:(hh + 1) * 128], lhsT=rqkT[:, 6 + h, :], rhs=rqkT[:, h, :], start=True, stop=True),
                             reads=[tg + "rqkT"], writes=["pb4"], mark=(hh == 2))
                    tt("dve", inT[:, hg * 3:(hg + 1) * 3, :], PB[4][:, 0:384].rearrange("p (h t) -> p h t", h=3),
                       C32("mask01T").unsqueeze(1).to_broadcast([P, 3, P]), ALU.mult, ["pb4", "c32"], [tg + "inT"])
                for h in range(6):
                    S.op("pe", lambda e, h=h: e.matmul(PB[5][:, h * 64:(h + 1) * 64], lhsT=inT[:, h, :], rhs=rvb[:, h * 64:(h + 1) * 64], start=True, stop=False),
                         reads=[tg + "inT", tg + "rvb"], writes=["pb5"], mark=False)
                    S.op("pe", lambda e, h=h: e.matmul(PB[5][:, h * 64:(h + 1) * 64], lhsT=rqkT[:, h, :], rhs=gS[:, h, :], start=False, stop=True),
                         reads=[tg + "rqkT", tg + "gS"], writes=["pb5"], mark=(h == 5))
                pump(2)
                for h in range(6):
                    S.op("pe", lambda e, h=h: e.matmul(PB[4][0:64, h * 64:(h + 1) * 64], lhsT=rqk[:, 384 + h * 64:384 + (h + 1) * 64], rhs=rvb[:, h * 64:(h + 1) * 64], start=True, stop=True),
                         reads=[tg + "rqk", tg + "rvb"], writes=["pb4"], mark=(h == 5))
                tt("dve", Stm[:], PB[4][0:64, 0:384].rearrange("p (h e) -> p h e", h=6),
                   C32("dec", 64)[:, 6:12].unsqueeze(2).to_broadcast([64, NH, HD]), ALU.mult, ["pb4", "c32"], [tg + "Stm"])
                tt("dve", Sst[:], Sst[:], C32("dec", 64)[:, 0:6].unsqueeze(2).to_broadcast([64, NH, HD]), ALU.mult, [tg + "S", "c32"], [tg + "S"])
                tt("dve", Sst[:], Sst[:], Stm[:], ALU.add, [tg + "S", tg + "Stm"], [tg + "S"])
                tt("dve", gS[:], Sst[:], C32("dec", 64)[:, 12:18].unsqueeze(2).to_broadcast([64, NH, HD]), ALU.mult, [tg + "S", "c32"], [tg + "gS"])
                if t == NT - 1:
                    S.dma("sp", lambda e: e.dma_start(out=rp[l].rearrange("h d e -> d h e"), in_=Sst[:]), reads=[tg + "S"], key="o_rp")
                group_norm(P, PB[5][:, 0:384], "pb5", osb, osq, st1, st2, st3, G[:, 0:384], mix[:, 0:384], tg, gkeys=[tg + "G0", tg + "G1"])
                pump(2)
                ubc = ub[t % 2]
                ubp = ub[(t + 1) % 2]
                S.op("act", lambda e, ubc=ubc: e.copy(out=ubc[:], in_=zs[:, O_PU:O_PU + 256]), reads=[tg + "zs"], writes=[tg + "ub%d" % (t % 2)])
                Acur = C16("A0") if t == 0 else C16("A1")
                for g in range(4):
                    S.op("pe", lambda e, g=g, ubc=ubc, Acur=Acur: e.matmul(PB[4][0:64, g * 128:(g + 1) * 128], lhsT=ubc[:, g * 64:(g + 1) * 64], rhs=Acur[:, g * 128:(g + 1) * 128], start=True, stop=(t == 0)),
                         reads=[tg + "ub%d" % (t % 2), "c16"], writes=["pb4"], mark=(t == 0 and g == 3))
                    if t > 0:
                        S.op("pe", lambda e, g=g, ubp=ubp: e.matmul(PB[4][0:64, g * 128:(g + 1) * 128], lhsT=ubp[:, g * 64:(g + 1) * 64], rhs=C16("AP")[:, g * 128:(g + 1) * 128], start=False, stop=True),
                             reads=[tg + "ub%d" % ((t + 1) % 2), "c16"], writes=["pb4"], mark=(g == 3))
                S.op("act", lambda e: e.copy(out=pT[0:64, :, :].rearrange("p g t -> p (g t)"), in_=PB[4][0:64, 0:512]), reads=["pb4"], writes=[tg + "pT"])
                for g in range(4):
                    S.op("pe", lambda e, g=g: e.matmul(PB[5][:, g * 64:(g + 1) * 64], lhsT=pT[0:65, g, :], rhs=pwa[0:65, g, :], start=True, stop=True),
                         reads=[tg + "pT", tg + "pT1", "pwa"], writes=["pb5"], mark=(g == 3))
                tt("dve", pos[:], PB[5][:, 0:256], psb[:], ALU.mult, ["pb5", "psb"], [tg + "pos"])
                tt("dve", mix[:, 768:1024], pos[:], G[:, 768:1024], ALU.mult, [tg + "pos", tg + "G0", tg + "G1"], [tg + "mixp"])
                pump(2)
                qk_norm(P, zs[:, O_MQ:O_MQ + 768], tg + "zs", None, osq_big=t1, mss=mss, tag=tg)
                rope(P, zs[:, O_MQ:O_MQ + 768], csb, t1[:], t2[:], rot[:], tg + "m", [tg + "zs", ck_], okey=tg + "rot")
                S.dma("sp", lambda e, t=t: e.dma_start(out=kp[l, t * 128:(t + 1) * 128, :], in_=rot[:, 384:768]), reads=[tg + "rot"], key="o_kp")
                S.op("dve", lambda e: e.tensor_copy(out=qaug[:, :, 0:64], in_=rot[:, 0:384].rearrange("p (h d) -> p h d", h=6)), reads=[tg + "rot"], writes=[tg + "qaug"])
                S.op("act", lambda e: e.copy(out=kb[:], in_=rot[:, 384:768]), reads=[tg + "rot"], writes=[tg + "kb"])
                S.op("act", lambda e, t=t: e.copy(out=vaug[:, t, :, 0:64], in_=zs[:, O_MV:O_MV + 384].rearrange("p (h d) -> p h d", h=6)), reads=[tg + "zs"], writes=[tg + "vaug%d" % t])
                pump(2)
                for h in range(6):
                    S.op("pe", lambda e, h=h: e.transpose(out=pbf(2)[0:64, h * 128:(h + 1) * 128], in_=kb[:, h * 64:(h + 1) * 64], identity=C16("ident")),
                         reads=[tg + "kb", "c16"], writes=["pb2"], mark=(h == 5))
                S.op("act", lambda e, t=t: e.copy(out=kTa[0:64, :, t * 128:(t + 1) * 128], in_=pbf(2)[0:64, 0:768].rearrange("p (h t) -> p h t", h=6)),
                     reads=["pb2"], writes=[tg + "kTa%d" % t])
                blk = t // 2
                if blk >= 4:
                    for h in range(6):
                        S.op("pe", lambda e, h=h: e.transpose(out=pbf(2)[0:64, h * 128:(h + 1) * 128], in_=qaug[:, h, 0:64], identity=C16("ident")),
                             reads=[tg + "qaug", "c16"], writes=["pb2"], mark=(h == 5))
                    S.op("dve", lambda e: e.tensor_copy(out=qTa[0:64, :, :].rearrange("p h t -> p (h t)"), in_=pbf(2)[0:64, 0:768]), reads=["pb2"], writes=[tg + "qTa"])
                    for h in range(6):
                        S.op("pe", lambda e, h=h: e.matmul(PB[4][:, h * 8:(h + 1) * 8], lhsT=qTa[0:64, h, :], rhs=kmT[:, h, :], start=True, stop=True),
                             reads=[tg + "qTa", tg + "kmT"], writes=["pb4"], mark=(h == 5))
                    if True:
                        S.op("dve", lambda e: e.memset(gsb[:].rearrange("p h c -> p (h c)"), -1e30), writes=[tg + "gsb"])
                        S.op("dve", lambda e, blk=blk: e.tensor_copy(out=gsb[:, :, 0:blk], in_=PB[4][:, 0:48].rearrange("p (h c) -> p h c", h=6)[:, :, 0:blk]),
                             reads=["pb4"], writes=[tg + "gsb"])
                        for h in range(6):
                            S.op("dve", lambda e, h=h: e.max(out=m8[:, h, :], in_=gsb[:, h, :]), reads=[tg + "gsb"], writes=[tg + "m8"])
                        tt("dve", sel[:], gsb[:], m8[:, :, 2:3].to_broadcast([P, 6, 8]), ALU.is_ge, [tg + "gsb", tg + "m8"], [tg + "sel"])
                        ts("dve", sel[:], sel[:], -1.0, -NEG, ALU.add, ALU.mult, [tg + "sel"], [tg + "sel"])
                        S.op("dve", lambda e, blk=blk: e.tensor_copy(out=qaug[:, :, 64:64 + blk], in_=sel[:, :, 0:blk]), reads=[tg + "sel"], writes=[tg + "qaugm"])
                pump(2)
                for h in range(6):
                    S.op("pe", lambda e, h=h: e.transpose(out=pbf(2)[0:72, h * 128:(h + 1) * 128], in_=qaug[:, h, :], identity=C16("ident")),
                         reads=[tg + "qaug", tg + "qaugm", "c16"], writes=["pb2"], mark=(h == 5))
                S.op("act", lambda e: e.copy(out=qTa[:].rearrange("p h t -> p (h t)"), in_=pbf(2)[0:72, 0:768]), reads=["pb2"], writes=[tg + "qTa"])
                if t % 2 == 1:
                    S.op("dve", lambda e, blk=blk: e.tensor_reduce(out=kmf[:, :, blk], in_=kTa[0:64, :, blk * 256:(blk + 1) * 256], axis=AX.X, op=ALU.add),
                         reads=[tg + "kTa%d" % (2 * blk), tg + "kTa%d" % (2 * blk + 1)], writes=[tg + "kmf"])
                    S.op("dve", lambda e, blk=blk: e.tensor_copy(out=kmT[:, :, blk], in_=kmf[:, :, blk]), reads=[tg + "kmf"], writes=[tg + "kmT"])
                pump(2)
                groups = []
                for h in range(6):
                    for g0 in range(0, t + 1, 4):
                        groups.append((h, list(range(g0, min(g0 + 4, t + 1)))))

                def emit_qk(i):
                    h, kts = groups[i]
                    bank = (3, 6)[i % 2]
                    for j, kt in enumerate(kts):
                        diag = (kt == t)
                        S.op("pe", lambda e, h=h, j=j, kt=kt, bank=bank, diag=diag: e.matmul(PB[bank][:, j * 128:(j + 1) * 128], lhsT=kTa[0:72, h, kt * 128:(kt + 1) * 128], rhs=qTa[0:72, h, :], start=True, stop=(not diag)),
                             reads=[tg + "kTa%d" % kt, tg + "kTaI", tg + "qTa"], writes=["pb%d" % bank], mark=(j == len(kts) - 1 and not diag))
                        if diag:
                            S.op("pe", lambda e, j=j, bank=bank: e.matmul(PB[bank][:, j * 128:(j + 1) * 128], lhsT=C16("ident"), rhs=C16("trineg"), start=False, stop=True),
                                 reads=["c16"], writes=["pb%d" % bank], mark=True)

                emit_qk(0)
                for i, (h, kts) in enumerate(groups):
                    if i + 1 < len(groups):
                        emit_qk(i + 1)
                    if i % max(1, len(groups) // 6) == 0:
                        pump(2)
                    bank = (3, 6)[i % 2]
                    ptb = PT[i % 2]
                    ptk = tg + "PT%d" % (i % 2)
                    nk = len(kts)
                    S.op("act", lambda e, nk=nk, bank=bank, ptb=ptb: e.activation(out=ptb[:, 0:nk, :].rearrange("p a q -> p (a q)"), in_=PB[bank][:, 0:nk * 128], func=AF.Exp, scale=0.125),
                         reads=["pb%d" % bank], writes=[ptk], c=0.12 + 0.13 * nk)
                    for j, kt in enumerate(kts):
                        S.op("pe", lambda e, h=h, j=j, kt=kt, ptb=ptb: e.matmul(PB[5][:, h * 65:(h + 1) * 65], lhsT=ptb[:, j, :], rhs=vaug[:, kt, h, :], start=(kt == 0), stop=(kt == t)),
                             reads=[ptk, tg + "vaug%d" % kt, tg + "vaug1"], writes=["pb5"], mark=(j == len(kts) - 1))
                pvv = PB[5][:, 0:390].rearrange("p (h e) -> p h e", h=6)
                S.op("dve", lambda e: e.reciprocal(out=rden[:].unsqueeze(2), in_=pvv[:, :, 64:65]), reads=["pb5"], writes=[tg + "rden"])
                tt("dve", osb[:].rearrange("p (h e) -> p h e", h=6), pvv[:, :, 0:64], rden[:].unsqueeze(2).to_broadcast([P, 6, 64]), ALU.mult, ["pb5", tg + "rden"], [tg + "osb"])
                tt("dve", mix[:, 384:768], osb[:], G[:, 384:768], ALU.mult, [tg + "osb", tg + "G0", tg + "G1"], [tg + "mixm"])
                pump(2)
                for kc in range(8):
                    S.op("pe", lambda e, kc=kc: e.transpose(out=pbf(2)[:, kc * 128:(kc + 1) * 128], in_=mix[:, kc * 128:(kc + 1) * 128], identity=C16("ident")),
                         reads=[tg + "mixr", tg + "mixm", tg + "mixp", "c16"], writes=["pb2"], mark=(kc == 7))
                S.op("dve", lambda e: e.tensor_copy(out=hT[:].rearrange("p k t -> p (k t)"), in_=pbf(2)[:, 0:1024]), reads=["pb2"], writes=[tg + "hT"])
                ytb = xb
                yk = xk
                for n in range(2):
                    for kc in range(8):
                        S.op("pe", lambda e, kc=kc, n=n: e.matmul(PB[n][:, :], lhsT=hT[:, kc, :], rhs=wout[:, kc, n * 512:(n + 1) * 512], start=(kc == 0), stop=(kc == 7)),
                             reads=[tg + "hT", "wout"], writes=["pb%d" % n], mark=(kc == 7), c=0.216)
                    tt("dve", ytb[:, n * 512:(n + 1) * 512], PB[n][:, :], xb[:, n * 512:(n + 1) * 512], ALU.add, ["pb%d" % n, xk], [yk])
                dst = y1s if l == 0 else yp
                S.dma("sp", lambda e, t=t, ytb=ytb, dst=dst: e.dma_start(out=dst[t * 128:(t + 1) * 128, :], in_=ytb[:]),
                      reads=[yk], writes=(["y1s"] if l == 0 else []), key=("dy1" if l == 0 else "o_yp"))

        for l in range(DEPTH):
            load_weights(l)
            with ExitStack() as kst:
                gen = sample_layer(l, kst)
                assert next(gen) == "pre"
                state = {"done": False}

                def pump(n):
                    for _ in range(n):
                        if state["done"]:
                            return
                        if next(gen) == "pages":
                            state["done"] = True

                with ExitStack() as ps_:
                    prompt_layer(l, ps_, pump)
                    while not state["done"]:
                        pump(1)
                    S.barrier()
                assert next(gen) == "post"
        S.emit()
    return nc


_CACHE = {}


def kernel(x_prompt, x_sample, cache_k, cache_v, state_ret, state_pool, page_table,
           norm_w, w_in, w_out, ret_gn_w, q_norm_w, k_norm_w, pool_w, pool_b, pool_scale):
    f = lambda a: np.ascontiguousarray(np.asarray(a, dtype=np.float32))
    x_prompt, x_sample, cache_k, cache_v = f(x_prompt), f(x_sample), f(cache_k), f(cache_v)
    state_ret, state_pool = f(state_ret), f(state_pool)
    page_table = np.ascontiguousarray(np.asarray(page_table, dtype=np.int32))
    if "c" not in _CACHE:
        _CACHE["c"] = _consts()
    o16, a16, o32, a32, (rope_p, blkind) = _CACHE["c"]
    nc = build_nc(o16, a16.shape[1], o32, a32.shape[1])
    ckr = cache_k.reshape(DEPTH, NPOOL, 128, 384)
    cvr = cache_v.reshape(DEPTH, NPOOL, 128, 384)
    common = dict(ck=ckr, cv=cvr, norm_w=f(norm_w), w_in=f(w_in), w_out=f(w_out), gn_w=f(ret_gn_w),
                  qn_w=f(q_norm_w), kn_w=f(k_norm_w), pool_w=f(pool_w), pool_b=f(pool_b), pool_s=f(pool_scale),
                  c16=a16, c32=a32, rope_p=rope_p, blkind=blkind)
    in_maps = []
    for c in range(NCORES):
        m = dict(common)
        m["xp"] = x_prompt[c]
        m["xs"] = x_sample[4 * c:4 * c + 4].reshape(16, D)
        m["sret"] = np.ascontiguousarray(state_ret[:, 4 * c:4 * c + 4])
        m["spool"] = np.ascontiguousarray(state_pool[:, 4 * c:4 * c + 4].reshape(DEPTH, 60, 256))
        m["ptab"] = np.ascontiguousarray(page_table[4 * c:4 * c + 4].reshape(1, 256))
        in_maps.append(m)
    res = run_bass_kernel_spmd(nc, in_maps, core_ids=list(range(NCORES)))
    R = res.results
    y_p = np.stack([R[c]["yp"] for c in range(NCORES)]).reshape(8, SEQ, D)
    y_s = np.concatenate([R[c]["ys"].reshape(4, 4, D) for c in range(NCORES)], axis=0)
    k_p = np.stack([R[c]["kp"] for c in range(NCORES)], axis=1).reshape(DEPTH, 8, SEQ, NH, HD)
    v_p = np.stack([R[c]["vp"] for c in range(NCORES)], axis=1).reshape(DEPTH, 8, SEQ, NH, HD)
    k_s = np.concatenate([R[c]["ks"].reshape(DEPTH, 4, 4, NH, HD) for c in range(NCORES)], axis=1)
    v_s = np.concatenate([R[c]["vs"].reshape(DEPTH, 4, 4, NH, HD) for c in range(NCORES)], axis=1)
    r_p = np.stack([R[c]["rp"] for c in range(NCORES)], axis=1)
    r_s = np.concatenate([R[c]["rs"] for c in range(NCORES)], axis=1)
    p_p = np.stack([R[c]["pp"] for c in range(NCORES)], axis=1)
    p_s = np.concatenate([R[c]["pso"] for c in range(NCORES)], axis=1)
    outs = (y_p, y_s, k_p, v_p, k_s, v_s, r_p, r_s, p_p, p_s)
    return tuple(np.ascontiguousarray(o, dtype=np.float32) for o in outs)
```
